# Optimizing a Trainium2 kernel written in Bass

```python
import jax, jax.numpy as jnp
from jax import lax
import numpy as np


D_MODEL = 1024
BATCH = 8
SEQ = 2048
DEPTH = 2

N_META = 16
Q_BLOCK = 128
SB_HEADS = 4
SB_HEAD_DIM = 64
SB_WIDTH = SB_HEADS * SB_HEAD_DIM
MLA_HEADS = 8
MLA_NOPE_DIM = 64
MLA_ROPE_DIM = 32
MLA_V_DIM = 64
MLA_Q_RANK = 256
MLA_KV_RANK = 128
MLA_WIDTH = MLA_HEADS * MLA_V_DIM
ROPE_THETA = 10000.0
CONV_CH = 256
CONV_K = 31
MIX_WIDTH = SB_WIDTH + MLA_WIDTH + CONV_CH
IN_SIZES = (SB_WIDTH, SB_WIDTH, SB_WIDTH, MLA_Q_RANK, MLA_KV_RANK, MLA_ROPE_DIM, CONV_CH, CONV_CH)
N_IN = sum(IN_SIZES)
N_EXPERTS = 32
TOP_K = 4
D_FF = D_MODEL
SWIGLU_LIMIT = 7.0
SWIGLU_ALPHA = 1.702
EXPERT_BLOCK = 128
DEEPNORM_ALPHA = (2 * DEPTH) ** 0.25
DEEPNORM_BETA = (8 * DEPTH) ** -0.25
LN_EPS = 1e-5
RMS_EPS = 1e-6

kernel_name = "hymba_style_sb_mla_conformer_moe_deepnorm"


def layer_norm(x, g, b):
    xf = x.astype(jnp.float32)
    mu = jnp.mean(xf, axis=-1, keepdims=True)
    var = jnp.mean(jnp.square(xf - mu), axis=-1, keepdims=True)
    y = (xf - mu) * lax.rsqrt(var + LN_EPS) * g.astype(jnp.float32) + b.astype(jnp.float32)
    return y.astype(x.dtype)


def rms_norm(x, g):
    xf = x.astype(jnp.float32)
    y = xf * lax.rsqrt(jnp.mean(jnp.square(xf), axis=-1, keepdims=True) + RMS_EPS) * g.astype(jnp.float32)
    return y.astype(x.dtype)


def rope_tables(length, dtype):
    inv = 1.0 / (ROPE_THETA ** (jnp.arange(0, MLA_ROPE_DIM, 2, dtype=jnp.float32) / MLA_ROPE_DIM))
    ang = jnp.arange(length, dtype=jnp.float32)[:, None] * inv[None, :]
    return jnp.cos(ang).astype(dtype), jnp.sin(ang).astype(dtype)


def rotary(x, cos, sin):
    x1, x2 = jnp.split(x, 2, axis=-1)
    c = cos[None, :, None, :]
    s = sin[None, :, None, :]
    return jnp.concatenate([x1 * c - x2 * s, x1 * s + x2 * c], axis=-1)


def sweep_query_blocks(attend, q):
    b, nh, total, dq = q.shape
    s = total - N_META
    nb = s // Q_BLOCK
    out_meta = attend(q[:, :, :N_META], jnp.arange(N_META))
    q_blocks = q[:, :, N_META:].reshape(b, nh, nb, Q_BLOCK, dq).transpose(2, 0, 1, 3, 4)
    pos_blocks = (N_META + jnp.arange(s)).reshape(nb, Q_BLOCK)
    out_blocks = lax.map(lambda qp: attend(qp[0], qp[1]), (q_blocks, pos_blocks))
    dv = out_blocks.shape[-1]
    out_real = out_blocks.transpose(1, 2, 0, 3, 4).reshape(b, nh, s, dv)
    return jnp.concatenate([out_meta, out_real], axis=2)


def stick_breaking_attend(q, k, v, q_pos):
    k_pos = jnp.arange(k.shape[2])
    z = jnp.einsum('bhqd,bhkd->bhqk', q, k).astype(jnp.float32) * (SB_HEAD_DIM ** -0.5)
    mask = k_pos[None, :] < q_pos[:, None]
    log_stay = jnp.where(mask, jax.nn.log_sigmoid(-z), 0.0)
    log_stay_after = lax.cumsum(log_stay, axis=3, reverse=True) - log_stay
    w = jnp.where(mask, jnp.exp(jax.nn.log_sigmoid(z) + log_stay_after), 0.0)
    return jnp.einsum('bhqk,bhkd->bhqd', w.astype(v.dtype), v)


def causal_softmax_attend(q, k, v, q_pos):
    k_pos = jnp.arange(k.shape[2])
    s = jnp.einsum('bhqd,bhkd->bhqk', q, k).astype(jnp.float32) * ((MLA_NOPE_DIM + MLA_ROPE_DIM) ** -0.5)
    s = jnp.where(k_pos[None, :] <= q_pos[:, None], s, -jnp.inf)
    p = jax.nn.softmax(s, axis=-1)
    return jnp.einsum('bhqk,bhkd->bhqd', p.astype(v.dtype), v)


def causal_depthwise_conv(u, w, b):
    out = lax.conv_general_dilated(u, w[:, None, :].astype(u.dtype), window_strides=(1,),
                                   padding=[(CONV_K - 1, 0)],
                                   dimension_numbers=('NWC', 'WIO', 'NWC'),
                                   feature_group_count=u.shape[-1])
    return out + b.astype(u.dtype)


def hybrid_mixer(h, w_in, q_norm_g, w_uq, kv_norm_g, w_ukv, conv_w, conv_b, conv_ln_g, conv_ln_b,
                 grp_norm_g, w_out, cos, sin):
    B, L, _ = h.shape
    proj = h @ w_in
    sb_q, sb_k, sb_v, c_q, c_kv, k_pe, conv_a, conv_g = jnp.split(
        proj, np.cumsum(IN_SIZES)[:-1].tolist(), axis=-1)

    def heads(t, n):
        return t.reshape(B, L, n, -1).transpose(0, 2, 1, 3)

    q, k, v = heads(sb_q, SB_HEADS), heads(sb_k, SB_HEADS), heads(sb_v, SB_HEADS)
    sb_out = sweep_query_blocks(lambda qb, pos: stick_breaking_attend(qb, k, v, pos), q)
    sb_out = sb_out.transpose(0, 2, 1, 3).reshape(B, L, SB_WIDTH)

    qm = (rms_norm(c_q, q_norm_g) @ w_uq).reshape(B, L, MLA_HEADS, MLA_NOPE_DIM + MLA_ROPE_DIM)
    qm = jnp.concatenate([qm[..., :MLA_NOPE_DIM], rotary(qm[..., MLA_NOPE_DIM:], cos, sin)], axis=-1)
    kvm = (rms_norm(c_kv, kv_norm_g) @ w_ukv).reshape(B, L, MLA_HEADS, MLA_NOPE_DIM + MLA_V_DIM)
    k_nope, vm = kvm[..., :MLA_NOPE_DIM], kvm[..., MLA_NOPE_DIM:]
    k_rot = rotary(k_pe[:, :, None, :], cos, sin)
    km = jnp.concatenate([k_nope, jnp.broadcast_to(k_rot, (B, L, MLA_HEADS, MLA_ROPE_DIM))], axis=-1)
    qm, km, vm = qm.transpose(0, 2, 1, 3), km.transpose(0, 2, 1, 3), vm.transpose(0, 2, 1, 3)
    mla_out = sweep_query_blocks(lambda qb, pos: causal_softmax_attend(qb, km, vm, pos), qm)
    mla_out = mla_out.transpose(0, 2, 1, 3).reshape(B, L, MLA_WIDTH)

    u = conv_a * jax.nn.sigmoid(conv_g)
    c = causal_depthwise_conv(u, conv_w, conv_b)
    c = jax.nn.silu(layer_norm(c, conv_ln_g, conv_ln_b))

    g_sb, g_mla, g_conv = jnp.split(grp_norm_g, [SB_WIDTH, SB_WIDTH + MLA_WIDTH])
    y = jnp.concatenate([rms_norm(sb_out, g_sb), rms_norm(mla_out, g_mla), rms_norm(c, g_conv)], axis=-1)
    return y @ w_out


def moe_ffn(h, router_w, router_b, w_gate_up, b_gate_up, w_down, b_down):
    B, L, D = h.shape
    xt = h.reshape(-1, D)
    T = xt.shape[0]
    logits = (xt @ router_w + router_b).astype(jnp.float32)
    top_val, top_idx = lax.top_k(logits, TOP_K)
    gate = jax.nn.softmax(top_val, axis=-1)
    n_assign = T * TOP_K
    flat_e = top_idx.reshape(-1)
    flat_tok = jnp.arange(n_assign) // TOP_K
    flat_g = gate.reshape(-1)
    order = jnp.argsort(flat_e)
    e_sorted = flat_e[order]
    counts = jnp.bincount(flat_e, length=N_EXPERTS)
    padded = (counts + EXPERT_BLOCK - 1) // EXPERT_BLOCK * EXPERT_BLOCK
    start = jnp.cumsum(counts) - counts
    pend = jnp.cumsum(padded)
    pstart = pend - padded
    dest = pstart[e_sorted] + (jnp.arange(n_assign) - start[e_sorted])
    n_rows = -(-n_assign // EXPERT_BLOCK) * EXPERT_BLOCK + N_EXPERTS * EXPERT_BLOCK
    n_blk = n_rows // EXPERT_BLOCK
    row_tok = jnp.zeros((n_rows,), jnp.int32).at[dest].set(flat_tok[order])
    row_g = jnp.zeros((n_rows,), jnp.float32).at[dest].set(flat_g[order])
    blk_e = jnp.minimum(jnp.searchsorted(pend, jnp.arange(n_blk) * EXPERT_BLOCK, side='right'), N_EXPERTS - 1)
    xs = xt[row_tok].reshape(n_blk, EXPERT_BLOCK, D)

    def expert_block(args):
        xb, e = args
        gu = xb @ w_gate_up[e] + b_gate_up[e]
        g, up = gu[:, :D_FF], gu[:, D_FF:]
        g = jnp.minimum(g, SWIGLU_LIMIT)
        up = jnp.clip(up, -SWIGLU_LIMIT, SWIGLU_LIMIT)
        act = (up + 1.0) * (g * jax.nn.sigmoid(SWIGLU_ALPHA * g))
        return act @ w_down[e] + b_down[e]

    ys = lax.map(expert_block, (xs, blk_e)).reshape(n_rows, D)
    out = jnp.zeros_like(xt).at[row_tok].add(ys * row_g[:, None].astype(ys.dtype))
    return out.reshape(B, L, D)


def setup_inputs(seed: int = 0) -> dict:
    key = jax.random.key(seed)
    ks = jax.random.split(key, 25)
    f32 = jnp.float32

    def nrm(k, shape, scale):
        return jax.random.normal(k, shape, f32) * scale

    def gain(k, shape):
        return 1.0 + 0.05 * jax.random.normal(k, shape, f32)

    def bias(k, shape, scale=0.02):
        return scale * jax.random.normal(k, shape, f32)

    return {
        "x": nrm(ks[0], (BATCH, SEQ, D_MODEL), 1.0),
        "meta_tokens": nrm(ks[1], (N_META, D_MODEL), 1.0),
        "ln_in_g": gain(ks[2], (D_MODEL,)),
        "ln_in_b": bias(ks[3], (D_MODEL,)),
        "w_in": nrm(ks[4], (DEPTH, D_MODEL, N_IN), D_MODEL ** -0.5),
        "q_norm_g": gain(ks[5], (DEPTH, MLA_Q_RANK)),
        "w_uq": nrm(ks[6], (DEPTH, MLA_Q_RANK, MLA_HEADS * (MLA_NOPE_DIM + MLA_ROPE_DIM)), MLA_Q_RANK ** -0.5),
        "kv_norm_g": gain(ks[7], (DEPTH, MLA_KV_RANK)),
        "w_ukv": nrm(ks[8], (DEPTH, MLA_KV_RANK, MLA_HEADS * (MLA_NOPE_DIM + MLA_V_DIM)), MLA_KV_RANK ** -0.5),
        "conv_w": nrm(ks[9], (DEPTH, CONV_K, CONV_CH), CONV_K ** -0.5),
        "conv_b": bias(ks[10], (DEPTH, CONV_CH)),
        "conv_ln_g": gain(ks[11], (DEPTH, CONV_CH)),
        "conv_ln_b": bias(ks[12], (DEPTH, CONV_CH)),
        "grp_norm_g": gain(ks[13], (DEPTH, MIX_WIDTH)),
        "w_out": nrm(ks[14], (DEPTH, MIX_WIDTH, D_MODEL), MIX_WIDTH ** -0.5 * DEEPNORM_BETA),
        "ln_mix_g": gain(ks[15], (DEPTH, D_MODEL)),
        "ln_mix_b": bias(ks[16], (DEPTH, D_MODEL)),
        "router_w": nrm(ks[17], (DEPTH, D_MODEL, N_EXPERTS), D_MODEL ** -0.5),
        "router_b": bias(ks[18], (DEPTH, N_EXPERTS), 0.01),
        "w_gate_up": nrm(ks[19], (DEPTH, N_EXPERTS, D_MODEL, 2 * D_FF), D_MODEL ** -0.5),
        "b_gate_up": bias(ks[20], (DEPTH, N_EXPERTS, 2 * D_FF)),
        "w_down": nrm(ks[21], (DEPTH, N_EXPERTS, D_FF, D_MODEL), D_FF ** -0.5 * DEEPNORM_BETA),
        "b_down": bias(ks[22], (DEPTH, N_EXPERTS, D_MODEL)),
        "ln_ffn_g": gain(ks[23], (DEPTH, D_MODEL)),
        "ln_ffn_b": bias(ks[24], (DEPTH, D_MODEL)),
    }


def reference(x, meta_tokens, ln_in_g, ln_in_b, w_in, q_norm_g, w_uq, kv_norm_g, w_ukv, conv_w, conv_b,
              conv_ln_g, conv_ln_b, grp_norm_g, w_out, ln_mix_g, ln_mix_b, router_w, router_b,
              w_gate_up, b_gate_up, w_down, b_down, ln_ffn_g, ln_ffn_b):
    B = x.shape[0]
    meta = jnp.broadcast_to(meta_tokens[None].astype(x.dtype), (B, N_META, D_MODEL))
    h = layer_norm(jnp.concatenate([meta, x], axis=1), ln_in_g, ln_in_b)
    cos, sin = rope_tables(h.shape[1], h.dtype)
    for l in range(DEPTH):
        mix = hybrid_mixer(h, w_in[l], q_norm_g[l], w_uq[l], kv_norm_g[l], w_ukv[l], conv_w[l], conv_b[l],
                           conv_ln_g[l], conv_ln_b[l], grp_norm_g[l], w_out[l], cos, sin)
        h = layer_norm(DEEPNORM_ALPHA * h + mix, ln_mix_g[l], ln_mix_b[l])
        ffn = moe_ffn(h, router_w[l], router_b[l], w_gate_up[l], b_gate_up[l], w_down[l], b_down[l])
        h = layer_norm(DEEPNORM_ALPHA * h + ffn, ln_ffn_g[l], ln_ffn_b[l])
    return h[:, N_META:, :]
```

```python
import numpy as np
import concourse.bass as bass
import concourse.mybir as mybir
from concourse.bass_utils import run_bass_kernel_spmd

F32 = mybir.dt.float32
BF16 = mybir.dt.bfloat16
I32 = mybir.dt.int32
AF = mybir.ActivationFunctionType
ALU = mybir.AluOpType

D = 1024
SEQ = 2048
NMETA = 16
L = SEQ + NMETA
DEPTH = 2
NT = 17
TOFF = [0] + [NMETA + 128 * i for i in range(16)]
TSZ = [NMETA] + [128] * 16
NCH = 5
COFF = [0] + [NMETA + 512 * i for i in range(4)]
CSZ = [NMETA] + [512] * 4
CH_TILES = [[0]] + [list(range(4 * c + 1, 4 * c + 5)) for c in range(4)]
TILE_CH = [0] + [1 + i // 4 for i in range(16)]
NIN = 1696
NE = 32
CAP = 640
NQ = 320
NBLK = CAP // 128
EC = NE * CAP
ALPHA = float((2 * DEPTH) ** 0.25)
LN_EPS = 1e-5
RMS_EPS = 1e-6
SB_SCALE = 64 ** -0.5
MLA_SCALE = 96 ** -0.5
SAME_ENGINE_SYNC = True
DEBUG_NO_SCATTER = False
DLEVEL = 9
MLA_SKEW = 2
MLA_FILL = 0
SB_FILL = 0


class Sched:
    ENG = ("pe", "act", "dve", "pool", "sp")

    def __init__(self):
        self.q = {e: [] for e in self.ENG}
        self.state = {}
        self.dma_cnt = {}
        self.bar = None
        self.bar_pending = set()

    def op(self, eng, fn, reads=(), writes=(), dma=None):
        deps = {}

        def add(ev):
            src = (ev[0], ev[1])
            if deps.get(src, -1) < ev[2]:
                deps[src] = ev[2]

        for k in reads:
            st = self.state.get(k)
            if st is not None and st[0] is not None:
                add(st[0])
        for k in writes:
            st = self.state.get(k)
            if st is not None:
                if st[0] is not None:
                    add(st[0])
                for ev in st[1].values():
                    add(ev)
        if eng in self.bar_pending:
            for src, v in self.bar.items():
                if deps.get(src, -1) < v:
                    deps[src] = v
            self.bar_pending.discard(eng)
        idx = len(self.q[eng])
        if dma is None:
            ev = ("E", eng, idx)
        else:
            self.dma_cnt[dma] = self.dma_cnt.get(dma, 0) + 1
            ev = ("D", dma, self.dma_cnt[dma])
        for k in reads:
            st = self.state.setdefault(k, [None, {}])
            st[1][(ev[0], ev[1])] = ev
        for k in writes:
            self.state[k] = [ev, {}]
        self.q[eng].append(dict(fn=fn, deps=deps, dma=dma, inc=False))

    def barrier(self):
        bar = {}
        for e in self.ENG:
            if self.q[e]:
                bar[("E", e)] = len(self.q[e]) - 1
        for k, c in self.dma_cnt.items():
            bar[("D", k)] = c
        self.bar = bar
        self.bar_pending = set(self.ENG)

    def _skip(self, kind, src, eng):
        return kind == "E" and src == eng and (eng == "pe" or eng == "sp" or not SAME_ENGINE_SYNC)

    def finalize(self):
        for eng in self.ENG:
            for rec in self.q[eng]:
                for (kind, src), v in rec["deps"].items():
                    if kind == "E" and not self._skip(kind, src, eng):
                        self.q[src][v]["inc"] = True
        self.val = {}
        for eng in self.ENG:
            c = 0
            vals = []
            for rec in self.q[eng]:
                if rec["inc"] and rec["dma"] is None:
                    c += 1
                vals.append(c)
            self.val[eng] = vals

    def emit(self, eng, e, esem, dsem):
        seen = {}
        for rec in self.q[eng]:
            for (kind, src), v in rec["deps"].items():
                if self._skip(kind, src, eng):
                    continue
                if kind == "E":
                    value = self.val[src][v]
                    sem = esem[src]
                else:
                    value = 16 * v
                    sem = dsem[src]
                if seen.get((kind, src), -1) >= value:
                    continue
                seen[(kind, src)] = value
                e.wait_ge(sem, value)
            if rec["fn"] is None:
                continue
            ins = rec["fn"](e)
            if rec["dma"] is not None:
                ins.then_inc(dsem[rec["dma"]], 16)
            elif rec["inc"]:
                ins.then_inc(esem[eng], 1)


class Arena:
    def __init__(self, nc, nbytes):
        self.t = nc.alloc_sbuf_tensor("arena", [128, nbytes // 2], BF16).ap()
        self.cap = nbytes
        self.off = 0
        self.peak = 0

    def alloc(self, free, dtype):
        esz = 2 if dtype == BF16 else 4
        n = int(np.prod(free))
        nb = (n * esz + 31) // 32 * 32
        assert self.off + nb <= self.cap, ("arena overflow", self.off, nb, self.cap)
        a = self.t[:, self.off // 2:(self.off + n * esz) // 2]
        if esz == 4:
            a = a.bitcast(dtype)
        if len(free) == 2:
            a = a.rearrange("p (a b) -> p a b", a=free[0])
        elif len(free) == 3:
            a = a.rearrange("p (a b c) -> p a b c", a=free[0], b=free[1])
        self.off += nb
        self.peak = max(self.peak, self.off)
        return a


PARAM_SPECS = [
    ("meta_tokens", [16, 1024]), ("ln_in_g", [1024]), ("ln_in_b", [1024]),
    ("w_in", [2, 1024, 1696]), ("q_norm_g", [2, 256]), ("w_uq", [2, 256, 768]),
    ("kv_norm_g", [2, 128]), ("w_ukv", [2, 128, 1024]), ("conv_w", [2, 31, 256]),
    ("conv_b", [2, 256]), ("conv_ln_g", [2, 256]), ("conv_ln_b", [2, 256]),
    ("grp_norm_g", [2, 1024]), ("w_out", [2, 1024, 1024]), ("ln_mix_g", [2, 1024]),
    ("ln_mix_b", [2, 1024]), ("router_w", [2, 1024, 32]), ("router_b", [2, 32]),
    ("w_gate_up", [2, 32, 1024, 2048]), ("b_gate_up", [2, 32, 2048]),
    ("w_down", [2, 32, 1024, 1024]), ("b_down", [2, 32, 1024]),
    ("ln_ffn_g", [2, 1024]), ("ln_ffn_b", [2, 1024]),
]


def host_consts():
    c = {}
    c["c_ident"] = np.eye(128, dtype=np.float32)
    p = np.arange(128)
    c["c_tris"] = -(p[:, None] >= p[None, :]).astype(np.float32)
    c["c_masks"] = (p[:, None] < p[None, :]).astype(np.float32)
    c["c_maskle"] = (p[:, None] <= p[None, :]).astype(np.float32)
    inv = 1.0 / (10000.0 ** (np.arange(0, 32, 2, dtype=np.float32) / 32.0))
    ang = np.arange(L, dtype=np.float32)[None, :] * inv[:, None].astype(np.float32)
    cs = np.cos(ang).astype(np.float32)
    sn = np.sin(ang).astype(np.float32)
    c["c_cos2"] = np.concatenate([cs, cs], 0)
    c["c_sin2"] = np.concatenate([-sn, sn], 0)
    c["c_ecol"] = np.broadcast_to((np.arange(NE, dtype=np.float32) * CAP - EC)[None, :], (128, NE)).copy()
    return c


def build_program(taps=None, stop=None):
    taps = taps or {}
    nc = bass.Bass("TRN2", target_bir_lowering=False)
    S = Sched()
    dt_in = {}
    x_d = nc.dram_tensor("x", [SEQ, D], F32, kind="ExternalInput").ap()
    out_d = nc.dram_tensor("out", [SEQ, D], F32, kind="ExternalOutput").ap()
    P = {}
    for name, shp in PARAM_SPECS:
        P[name] = nc.dram_tensor(name, shp, F32, kind="ExternalInput").ap()
    CD = {}
    for name, arr in host_consts().items():
        CD[name] = nc.dram_tensor(name, list(arr.shape), F32, kind="ExternalInput").ap()
    tap_d = {}
    for name, shp in taps.items():
        tap_d[name] = nc.dram_tensor("tap_" + name, list(shp), F32, kind="ExternalOutput").ap()
    hres = nc.dram_tensor("hres", [L, D], F32).ap()
    Xs = nc.dram_tensor("xs_scr", [EC + 128, D], BF16).ap()
    Ys = nc.dram_tensor("ys_scr", [EC + 128, D], BF16).ap()

    AR = Arena(nc, 206 * 1024)
    ps = [nc.alloc_psum_tensor("ps%d" % i, [128, 512], F32).ap() for i in range(7)]
    psb32 = nc.alloc_psum_tensor("psb", [128, 512], F32).ap()
    psb = psb32[:, :].bitcast(BF16)
    PSK = ["ps%d" % i for i in range(7)]

    ident = AR.alloc([128], F32)
    identb = AR.alloc([128], BF16)
    tris = AR.alloc([128], BF16)
    masks = AR.alloc([128], BF16)
    maskle = AR.alloc([128], BF16)
    c1 = AR.alloc([128], BF16)
    c128 = AR.alloc([128], BF16)
    c256 = AR.alloc([128], BF16)
    c512 = AR.alloc([128], BF16)
    cm1 = AR.alloc([128], BF16)
    silub = AR.alloc([8], F32)
    zbig = AR.alloc([2, D], BF16)
    zer = AR.alloc([512], BF16)
    ecol = AR.alloc([NE], F32)
    U = AR.alloc([8, L], BF16)
    lng = AR.alloc([D], F32)
    lnb = AR.alloc([D], F32)
    Gt = AR.alloc([NT, NE], F32)
    ridi = AR.alloc([NT, 4], I32)
    gk = AR.alloc([NT, 4], F32)
    PERSIST = AR.off

    def UK(t):
        return ("U", t)

    UALL = [UK(t) for t in range(NT)]

    def ukeys(c):
        return [UK(t) for t in CH_TILES[c]]

    cnt = [0]

    def uid():
        cnt[0] += 1
        return cnt[0]

    def dma(eng, out, in_, reads, writes, sem):
        S.op(eng, lambda e: e.dma_start(out=out, in_=in_), reads, writes, dma=sem)

    def mm(out, lhsT, rhs, start, stop, reads, writes):
        S.op("pe", lambda e: e.matmul(out, lhsT, rhs, start=start, stop=stop), reads, writes)

    def tr(out, in_, idn, reads, writes):
        S.op("pe", lambda e: e.transpose(out, in_, idn), reads, writes)

    def act(out, in_, func, reads, writes, bias=0.0, scale=1.0, eng="act"):
        S.op("act", lambda e: e.activation(out=out, in_=in_, func=func, bias=bias, scale=scale), reads, writes)

    def tt(eng, out, a, b, op, reads, writes):
        S.op(eng, lambda e: e.tensor_tensor(out=out, in0=a, in1=b, op=op), reads, writes)

    def ts(eng, out, a, s1, s2, op0, op1, reads, writes):
        if op1 is None:
            S.op(eng, lambda e: e.tensor_scalar(out=out, in0=a, scalar1=s1, scalar2=None, op0=op0), reads, writes)
        else:
            S.op(eng, lambda e: e.tensor_scalar(out=out, in0=a, scalar1=s1, scalar2=s2, op0=op0, op1=op1), reads, writes)

    def stt(out, a, sc, b, op0, op1, reads, writes, accum=None):
        if accum is None:
            S.op("dve", lambda e: e.scalar_tensor_tensor(out=out, in0=a, scalar=sc, in1=b, op0=op0, op1=op1), reads, writes)
        else:
            S.op("dve", lambda e: e.scalar_tensor_tensor(out=out, in0=a, scalar=sc, in1=b, op0=op0, op1=op1, accum_out=accum), reads, writes)

    def cp(eng, out, in_, reads, writes):
        if eng == "act":
            S.op("act", lambda e: e.copy(out=out, in_=in_), reads, writes)
        else:
            S.op(eng, lambda e: e.tensor_copy(out=out, in_=in_), reads, writes)

    def rsqrt(out, in_, tmp, eps, reads, writes, tmpkey):
        act(tmp, in_, AF.Ln, reads, [tmpkey], bias=eps, scale=1.0)
        act(out, tmp, AF.Exp, [tmpkey], writes, scale=-0.5)

    def tap(name, src_ap, dst_slice, reads):
        if name in tap_d:
            dma("pool", dst_slice(tap_d[name]), src_ap, reads, ["tap_" + name], "tap_" + name)

    mark0 = AR.off
    stg = AR.alloc([128], F32)
    for nm, dst, val in (("c_tris", tris, None), ("c_masks", masks, None), ("c_maskle", maskle, None), ("c_ident", identb, None)):
        dma("sp", stg[:, :], CD[nm], [], ["stg"], "stg")
        cp("dve", dst[:, :], stg[:, :], ["stg"], ["const"])
    dma("sp", ident[:, :], CD["c_ident"], [], ["const"], "stg")
    dma("sp", ecol[:, :], CD["c_ecol"], [], ["const"], "stg")
    for dst, v in ((c1, 1.0), (c128, 1.0 / 128), (c256, 1.0 / 256), (c512, 1.0 / 512), (cm1, -1.0)):
        S.op("pool", lambda e, dst=dst, v=v: e.memset(dst[:, :], v), [], ["const"])
    S.op("pool", lambda e: e.memset(zer[:, :], 0.0), [], ["const"])
    S.op("pool", lambda e: e.memset(silub[:, :], 7.0 * 1.702), [], ["const"])
    S.op("pool", lambda e: e.memset(zbig[:, :, :], 0.0), [], ["const"])
    zrow = AR.alloc([D], BF16)
    S.op("pool", lambda e: e.memset(zrow[:, :], 0.0), [], ["zrow"])
    dma("sp", Ys[EC:EC + 128, :], zrow[:, :], ["zrow"], ["Ys_dummy"], "zrow")
    S.barrier()
    AR.off = mark0
    nz = (EC + 128) // 256
    for i in range(nz):
        dma("pool", Xs[i * 256:(i + 1) * 256, :].rearrange("(p a) d -> p a d", a=2), zbig[:, :, :], ["const"], [("Xs0", i)], "zbig")
    if (EC + 128) % 256:
        dma("pool", Xs[nz * 256:EC + 128, :], zbig[:, 0, :], ["const"], [("Xs0", nz)], "zbig")

    def load_ln(gap, bap):
        dma("sp", lng[:, :], gap.partition_broadcast(128), [], ["lng"], "lng")
        dma("sp", lnb[:, :], bap.partition_broadcast(128), [], ["lnb"], "lnb")

    def layer_norm(src, dst, sz, st6, mv, skey, dkey, tmpkey):
        ln_stats(src, sz, st6, mv, skey, tmpkey)
        ln_apply(src, dst, sz, mv, skey, dkey, tmpkey)

    def ln_apply(src, dst, sz, mv, skey, dkey, tmpkey):
        act(dst[0:sz, :], src[0:sz, :], AF.Identity, [skey, tmpkey], [dkey], bias=mv[0:sz, 3:4], scale=mv[0:sz, 2:3])
        tt("dve", dst[0:sz, :], dst[0:sz, :], lng[0:sz, :], ALU.mult, [dkey, "lng"], [dkey])
        tt("dve", dst[0:sz, :], dst[0:sz, :], lnb[0:sz, :], ALU.add, [dkey, "lnb"], [dkey])

    def ln_stats(src, sz, st6, mv, skey, tmpkey):
        S.op("dve", lambda e: e.bn_stats(out=st6[0:sz, 0:6], in_=src[0:sz, 0:512]), [skey], [tmpkey])
        S.op("dve", lambda e: e.bn_stats(out=st6[0:sz, 6:12], in_=src[0:sz, 512:1024]), [skey], [tmpkey])
        S.op("dve", lambda e: e.bn_aggr(out=mv[0:sz, 0:2], in_=st6[0:sz, 0:12]), [tmpkey], [tmpkey])
        rsqrt(mv[0:sz, 2:3], mv[0:sz, 1:2], mv[0:sz, 4:5], LN_EPS, [tmpkey], [tmpkey], tmpkey)
        stt(mv[0:sz, 3:4], mv[0:sz, 0:1], -1.0, mv[0:sz, 2:3], ALU.mult, ALU.mult, [tmpkey], [tmpkey])

    def transpose_to_U(src, sz, t, skey, want32=None, w32key=None):
        for half in range(2):
            pb = ps[5 + half]
            pk = PSK[5 + half]
            for kk in range(4):
                k = half * 4 + kk
                tr(pb[:, kk * 128:kk * 128 + sz], src[0:sz, k * 128:(k + 1) * 128], ident[0:sz, 0:sz], [skey, "const"], [pk])
            pv = pb[:, :].rearrange("p (k t) -> p k t", k=4)[:, :, 0:sz]
            if want32 is not None:
                S.op("dve", lambda e, pv=pv, half=half: e.tensor_copy(out=want32[:, half * 4:half * 4 + 4, 0:sz], in_=pv), [pk], [w32key])
                S.op("pool", lambda e, half=half: e.tensor_copy(out=U[:, half * 4:half * 4 + 4, TOFF[t]:TOFF[t] + sz],
                                                                 in_=want32[:, half * 4:half * 4 + 4, 0:sz]), [w32key], [UK(t)])
            else:
                S.op("dve", lambda e, pv=pv, half=half: e.tensor_copy(out=U[:, half * 4:half * 4 + 4, TOFF[t]:TOFF[t] + sz], in_=pv), [pk], [UK(t)])

    mark0 = AR.off
    xin = [AR.alloc([D], F32) for _ in range(2)]
    hn = [AR.alloc([D], F32) for _ in range(2)]
    st6 = [AR.alloc([12], F32) for _ in range(2)]
    mv = [AR.alloc([8], F32) for _ in range(2)]
    load_ln(P["ln_in_g"], P["ln_in_b"])
    def p0_front(t):
        s = t % 2
        sz = TSZ[t]
        src = P["meta_tokens"] if t == 0 else x_d[(t - 1) * 128:t * 128, :]
        dma("sp", xin[s][0:sz, :], src, [], ["xin%d" % s], "xin%d" % s)
        ln_stats(xin[s], sz, st6[s], mv[s], "xin%d" % s, "lnt%d" % s)

    def p0_back(t):
        s = t % 2
        sz = TSZ[t]
        ln_apply(xin[s], hn[s], sz, mv[s], "xin%d" % s, "hn%d" % s, "lnt%d" % s)
        dma("sp", hres[TOFF[t]:TOFF[t] + sz, :], hn[s][0:sz, :], ["hn%d" % s], [("hres", t)], "hn%d" % s)
        transpose_to_U(hn[s], sz, t, "hn%d" % s)
        if t == 1:
            tap("h0", hn[s][:, :], lambda d: d[0:128, :], ["hn%d" % s])

    for t in range(NT + 1):
        if t < NT:
            p0_front(t)
        if t >= 1:
            p0_back(t - 1)
    S.barrier()
    AR.off = mark0

    for l in range(DEPTH):
        last = l == DEPTH - 1
        markL = AR.off
        Qsb = AR.alloc([2, L], BF16)
        Ksb = AR.alloc([2, L], BF16)
        Vsb = AR.alloc([NT, 256], BF16)
        cqT = AR.alloc([2, L], BF16)
        ckvT = AR.alloc([L], BF16)
        krot = AR.alloc([L], BF16)
        uT = AR.alloc([2, L + 30], BF16)
        rqb = AR.alloc([L], BF16)
        rkb = AR.alloc([L], BF16)
        rkp = AR.alloc([NT], F32)
        colv = AR.alloc([79], F32)
        gwq = AR.alloc([2, 8, 96], BF16)
        gwqs = AR.alloc([2, 8, 96], BF16)
        gwk = AR.alloc([8, 64], BF16)
        gwv = AR.alloc([8, 64], BF16)
        convd = AR.alloc([2, 31, 128], BF16)
        cos2 = AR.alloc([L], BF16)
        sin2 = AR.alloc([L], BF16)
        for nm, dst in (("c_cos2", cos2), ("c_sin2", sin2)):
            for hf in range(2):
                dma("pool", dst[64:96, hf * 1032:(hf + 1) * 1032], CD[nm][:, hf * 1032:(hf + 1) * 1032], [], ["const"], "cs2")
        markA = AR.off
        win = AR.alloc([8, NIN], BF16)
        wkpe = AR.alloc([8, 2, 96], BF16)
        vst = AR.alloc([128], F32)
        wq32 = AR.alloc([2, 768], F32)
        wkv32 = AR.alloc([1024], F32)
        sgt = [AR.alloc([512], F32) for _ in range(2)]
        sqt = [AR.alloc([512], BF16) for _ in range(3)]
        rt = [AR.alloc([512], F32) for _ in range(3)]

        dma("pool", win[:, :, :], P["w_in"][l].rearrange("(k p) n -> p k n", p=128), [], ["win"], "win")
        S.op("pool", lambda e: e.memset(wkpe[:, :, :, :], 0.0), [], ["wkpe"])
        w_in_v = P["w_in"][l].rearrange("(k p) n -> p k n", p=128)
        dma("pool", wkpe[:, :, 0, 64:96], w_in_v[:, :, 1152:1184], [], ["wkpe"], "wkpe")
        dma("pool", wkpe[:, :, 1, 64:80], w_in_v[:, :, 1168:1184], [], ["wkpe"], "wkpe")
        dma("pool", wkpe[:, :, 1, 80:96], w_in_v[:, :, 1152:1168], [], ["wkpe"], "wkpe")
        S.op("pool", lambda e: e.memset(vst[:, :], 0.0), [], ["vst"])
        rows = [("conv_b", 0, 2), ("conv_ln_g", 2, 2), ("conv_ln_b", 4, 2), ("grp_norm_g", 6, 8),
                ("q_norm_g", 14, 2), ("kv_norm_g", 16, 1)]
        for nm, r0, nr in rows:
            dma("sp", vst[r0:r0 + nr, :], P[nm][l].rearrange("(r p) -> r p", p=128), [], ["vst"], "vst")
        dma("sp", vst[17:79, :], P["conv_w"][l].rearrange("k (j p) -> (k j) p", p=128), [], ["vst"], "vst")
        tr(ps[6][:, 0:79], vst[0:79, :], ident[0:79, 0:79], ["vst", "const"], [PSK[6]])
        cp("dve", colv[:, :], ps[6][:, 0:79], [PSK[6]], ["colv"])
        CB, CLG, CLB, GG, QG, KG, CW = 0, 2, 4, 6, 14, 16, 17
        dma("sp", wq32[:, :, :], P["w_uq"][l].rearrange("(k p) n -> p k n", p=128), [], ["wq32"], "wq32")
        S.op("pool", lambda e: e.memset(gwqs[:, :, :, :], 0.0), [], ["gwqs"])
        for k in range(2):
            wv = wq32[:, k, :].rearrange("p (h c) -> p h c", h=8)
            ts("dve", gwq[:, k, :, :], wv, colv[:, QG + k:QG + k + 1], None, ALU.mult, None, ["wq32", "colv"], ["gwq"])
            ts("dve", gwqs[:, k, :, 64:80], wv[:, :, 80:96], colv[:, QG + k:QG + k + 1], None, ALU.mult, None, ["wq32", "colv", "gwqs"], ["gwqs"])
            ts("dve", gwqs[:, k, :, 80:96], wv[:, :, 64:80], colv[:, QG + k:QG + k + 1], None, ALU.mult, None, ["wq32", "colv", "gwqs"], ["gwqs"])
        dma("sp", wkv32[:, :], P["w_ukv"][l], [], ["wkv32"], "wkv32")
        wkv = wkv32[:, :].rearrange("p (h c) -> p h c", h=8)
        ts("dve", gwk[:, :, :], wkv[:, :, 0:64], colv[:, KG:KG + 1], None, ALU.mult, None, ["wkv32", "colv"], ["gwk"])
        ts("dve", gwv[:, :, :], wkv[:, :, 64:128], colv[:, KG:KG + 1], None, ALU.mult, None, ["wkv32", "colv"], ["gwv"])
        for j in range(2):
            for k in range(31):
                ts("dve", convd[:, j, k, :], identb[:, :], colv[:, CW + 2 * k + j:CW + 2 * k + j + 1], None, ALU.mult, None,
                   ["const", "colv"], ["convd"])
        S.op("pool", lambda e: e.memset(uT[:, :, 0:30], 0.0), [], ["uTpad"])

        rr = [0]

        def nextps(n=5):
            b = rr[0] % n
            rr[0] += 1
            return b

        def proj_fm(c, col0, M, lhs_fn=None):
            b = nextps()
            n = CSZ[c]
            for k in range(8):
                lhsT = win[:, k, col0:col0 + M] if lhs_fn is None else lhs_fn(k)
                mm(ps[b][0:M, 0:n], lhsT, U[:, k, COFF[c]:COFF[c] + n], k == 0, k == 7,
                   ["win", "wkpe"] + ukeys(c), [PSK[b]])
            return b

        for c in range(NCH):
            n = CSZ[c]
            cs = slice(COFF[c], COFF[c] + n)
            for j in range(2):
                b = proj_fm(c, j * 128, 128)
                cp("act", Qsb[:, j, cs], ps[b][:, 0:n], [PSK[b]], [("Qsb", c)])
                b = proj_fm(c, 256 + j * 128, 128)
                cp("act", Ksb[:, j, cs], ps[b][:, 0:n], [PSK[b]], [("Ksb", c)])
            for j in range(2):
                b = proj_fm(c, 768 + j * 128, 128)
                cp("act", cqT[:, j, cs], ps[b][:, 0:n], [PSK[b]], [("cqT", c)])
                tt("pool", sqt[j][:, 0:n], cqT[:, j, cs], cqT[:, j, cs], ALU.mult, [("cqT", c)], ["sqt%d" % j])
            b = nextps()
            for j in range(2):
                mm(ps[b][:, 0:n], c256[:, :], sqt[j][:, 0:n], j == 0, j == 1, ["const", "sqt%d" % j], [PSK[b]])
            rsqrt(rqb[:, cs], ps[b][:, 0:n], rt[2][:, 0:n], RMS_EPS, [PSK[b]], [("rqb", c)], "rt2")
            b = proj_fm(c, 1024, 128)
            cp("act", ckvT[:, cs], ps[b][:, 0:n], [PSK[b]], [("ckvT", c)])
            tt("pool", sqt[2][:, 0:n], ckvT[:, cs], ckvT[:, cs], ALU.mult, [("ckvT", c)], ["sqt2"])
            b = nextps()
            mm(ps[b][:, 0:n], c128[:, :], sqt[2][:, 0:n], True, True, ["const", "sqt2"], [PSK[b]])
            rsqrt(rkb[:, cs], ps[b][:, 0:n], rt[2][:, 0:n], RMS_EPS, [PSK[b]], [("rkb", c)], "rt2")
            b = nextps()
            for t in CH_TILES[c]:
                o = TOFF[t] - COFF[c]
                mm(ps[b][0:TSZ[t], t:t + 1], sqt[2][:, o:o + TSZ[t]], c128[:, 0:1], True, True, ["const", "sqt2"], [PSK[b]])
            for t in CH_TILES[c]:
                rsqrt(rkp[0:TSZ[t], t:t + 1], ps[b][0:TSZ[t], t:t + 1], rt[2][0:TSZ[t], 0:1], RMS_EPS, [PSK[b]], ["rkp"], "rt2")
            b1 = proj_fm(c, 0, 96, lambda k: wkpe[:, k, 0, :])
            b2 = proj_fm(c, 0, 96, lambda k: wkpe[:, k, 1, :])
            tt("dve", rt[0][64:96, 0:n], ps[b1][64:96, 0:n], cos2[64:96, cs], ALU.mult, [PSK[b1], "const"], ["rt0"])
            tt("dve", rt[1][64:96, 0:n], ps[b2][64:96, 0:n], sin2[64:96, cs], ALU.mult, [PSK[b2], "const"], ["rt1"])
            tt("pool", krot[64:96, cs], rt[0][64:96, 0:n], rt[1][64:96, 0:n], ALU.add, ["rt0", "rt1"], [("krot", c)])
            for j in range(2):
                ba = proj_fm(c, 1184 + j * 128, 128)
                bg = proj_fm(c, 1440 + j * 128, 128)
                act(sgt[j][:, 0:n], ps[bg][:, 0:n], AF.Sigmoid, [PSK[bg]], ["sgt%d" % j])
                tt("dve", uT[:, j, 30 + COFF[c]:30 + COFF[c] + n], ps[ba][:, 0:n], sgt[j][:, 0:n], ALU.mult,
                   [PSK[ba], "sgt%d" % j], [("uT", c)])
            for t in CH_TILES[c]:
                b = nextps()
                sz = TSZ[t]
                for k in range(8):
                    mm(ps[b][0:sz, 0:256], U[:, k, TOFF[t]:TOFF[t] + sz], win[:, k, 512:768], k == 0, k == 7,
                       ["win", UK(t)], [PSK[b]])
                cp("act", Vsb[0:sz, t, :], ps[b][0:sz, 0:256], [PSK[b]], [("Vsb", t)])
        if l == 0:
            tap("qsb", Qsb[:, 0, 16:528], lambda d: d[:, :], [("Qsb", 1)])
            tap("cos2", cos2[64:96, :], lambda d: d[:, :], ["const"])
            tap("sin2", sin2[64:96, :], lambda d: d[:, :], ["const"])
            tap("krot", krot[64:96, 16:528], lambda d: d[:, :], [("krot", 1)])
            tap("uT", uT[:, 0, 30 + 16:30 + 528], lambda d: d[:, :], [("uT", 1)])
            tap("rqb", rqb[:, 16:528], lambda d: d[:, :], [("rqb", 1)])
        S.barrier()
        if stop == "A":
            break
        AR.off = markA

        markSB = AR.off
        sg = [[AR.alloc([512], F32) for _ in range(3)] for _ in range(2)]
        nl = [[AR.alloc([512], BF16) for _ in range(3)] for _ in range(2)]
        ex = [[AR.alloc([512], F32) for _ in range(2)] for _ in range(2)]
        wt = [[AR.alloc([512], BF16) for _ in range(2)] for _ in range(2)]
        Aacc = [AR.alloc([512], BF16) for _ in range(2)]

        def chunk_steps(c, descending):
            steps = []
            for kt in range(0, CH_TILES[c][-1] + 1):
                if kt in CH_TILES[c]:
                    co = TOFF[kt] - COFF[c]
                    steps.append((kt, co, CSZ[c] - co, True))
                else:
                    steps.append((kt, 0, CSZ[c], False))
            return steps[::-1] if descending else steps

        it = [0]
        sb_steps = []
        for j in range(2):
            for c in range(NCH):
                steps = chunk_steps(c, True)
                ob = it[0] % 2
                it[0] += 1
                for si, st_ in enumerate(steps):
                    sb_steps.append((j, c, ob, si, len(steps), st_))
        OB = [ps[6], psb32]
        OBK = [PSK[6], "psb"]

        def sb_stage_a(gi):
            j, c, ob, si, ns, (kt, co, n2, diag) = sb_steps[gi]
            sz = TSZ[kt]
            s3 = gi % 3
            kc = TILE_CH[kt]
            qs = slice(COFF[c] + co, COFF[c] + co + n2)
            for hp in range(2):
                pr = slice(hp * 64, hp * 64 + 64)
                zb = (gi % 2) * 2 + hp
                mm(ps[zb][0:sz, 0:n2], Ksb[pr, j, TOFF[kt]:TOFF[kt] + sz], Qsb[pr, j, qs], True, True,
                   [("Ksb", kc), ("Qsb", c)], [PSK[zb]])
            for hp in range(2):
                zb = (gi % 2) * 2 + hp
                k_ = "%d_%d" % (hp, s3)
                act(sg[hp][s3][0:sz, 0:n2], ps[zb][0:sz, 0:n2], AF.Exp, [PSK[zb]], ["sg" + k_], scale=SB_SCALE)
                if diag:
                    w_ = min(sz, 128)
                    tt("pool", sg[hp][s3][0:sz, 0:w_], sg[hp][s3][0:sz, 0:w_], masks[0:sz, 0:w_], ALU.mult,
                       ["sg" + k_, "const"], ["sg" + k_])
                act(nl[hp][s3][0:sz, 0:n2], sg[hp][s3][0:sz, 0:n2], AF.Ln, ["sg" + k_], ["nl" + k_], bias=1.0, scale=1.0)

        def sb_stage_b(gi):
            j, c, ob, si, ns, (kt, co, n2, diag) = sb_steps[gi]
            n = CSZ[c]
            sz = TSZ[kt]
            s3 = gi % 3
            s2 = gi % 2
            O = OB[ob]
            first = si == 0
            if first:
                mm(O[:, 0:n], zer[:, 0:128], zer[:, 0:n], True, False, ["const"], [OBK[ob]])
                for hp in range(2):
                    S.op("pool", lambda e, hp=hp: e.memset(Aacc[hp][:, :], 0.0), [], ["Aacc%d" % hp])
            for hp in range(2):
                cb = 4 + hp
                k_ = "%d_%d" % (hp, s3)
                mm(ps[cb][0:sz, 0:n2], tris[0:sz, 0:sz], nl[hp][s3][0:sz, 0:n2], True, first, ["const", "nl" + k_], [PSK[cb]])
                if not first:
                    mm(ps[cb][0:sz, 0:n2], cm1[:, 0:sz], Aacc[hp][:, co:co + n2], False, True, ["const", "Aacc%d" % hp], [PSK[cb]])
            for hp in range(2):
                cb = 4 + hp
                k_ = "%d_%d" % (hp, s3)
                k2 = "%d_%d" % (hp, s2)
                act(ex[hp][s2][0:sz, 0:n2], ps[cb][0:sz, 0:n2], AF.Exp, [PSK[cb]], ["ex" + k2])
                tt("dve", wt[hp][s2][0:sz, 0:n2], sg[hp][s3][0:sz, 0:n2], ex[hp][s2][0:sz, 0:n2], ALU.mult,
                   ["sg" + k_, "ex" + k2], ["wt" + k2])
                if si != ns - 1:
                    tt("pool", Aacc[hp][:, co:co + n2], Aacc[hp][:, co:co + n2], nl[hp][s3][:, 0:n2], ALU.add,
                       ["Aacc%d" % hp, "nl" + k_], ["Aacc%d" % hp])

        def sb_stage_c(gi):
            j, c, ob, si, ns, (kt, co, n2, diag) = sb_steps[gi]
            n = CSZ[c]
            sz = TSZ[kt]
            s2 = gi % 2
            O = OB[ob]
            for hp in range(2):
                h = 2 * j + hp
                k2 = "%d_%d" % (hp, s2)
                last_mm = si == ns - 1
                S.op("pe", lambda e, O=O, hp=hp, sz=sz, kt=kt, h=h, co=co, n2=n2, s2=s2, last_mm=last_mm: e.matmul(
                    O[hp * 64:hp * 64 + 64, co:co + n2], Vsb[0:sz, kt, h * 64:(h + 1) * 64], wt[hp][s2][0:sz, 0:n2],
                    start=False, stop=last_mm, tile_position=(0, hp * 64)),
                    [("Vsb", kt), "wt" + k2], [OBK[ob]])
            if si == ns - 1:
                cp("act", U[:, j, COFF[c]:COFF[c] + n], O[:, 0:n], [OBK[ob]], ukeys(c))

        for gi in range(len(sb_steps) + 2):
            if gi < len(sb_steps):
                sb_stage_a(gi)
            if 1 <= gi <= len(sb_steps):
                sb_stage_b(gi - 1)
            if gi >= 2:
                sb_stage_c(gi - 2)
        S.barrier()
        AR.off = markSB
        pt = [AR.alloc([512], BF16) for _ in range(3)]
        rec = [AR.alloc([512], F32) for _ in range(2)]
        rt = [AR.alloc([512], F32) for _ in range(2)]
        QmT = [AR.alloc([L], BF16) for _ in range(2)]
        KmT = [AR.alloc([L], BF16) for _ in range(2)]
        Vmh = [AR.alloc([NT, 128], BF16) for _ in range(2)]
        for i in range(2):
            S.op("pool", lambda e, i=i: e.memset(Vmh[i][:, :, 64:128], 1.0), [], ["Vmh%d" % i])
        if l == 0:
            tap("sbo", U[:, 0, 16:528], lambda d: d[:, :], ukeys(1))
        if stop == "SB":
            break

        def mla_jit_tasks(h):
            hb = h % 2
            Q, K, V = QmT[hb], KmT[hb], Vmh[hb]
            qk, kk_, vk = "QmT%d" % hb, "KmT%d" % hb, "Vmh%d" % hb
            tasks = []

            def qk_task(c):
                n = CSZ[c]
                cs = slice(COFF[c], COFF[c] + n)
                b1, b2 = 6, 3
                for k in range(2):
                    mm(ps[b1][0:96, 0:n], gwq[:, k, h, :], cqT[:, k, cs], k == 0, k == 1, ["gwq", ("cqT", c)], [PSK[b1]])
                for k in range(2):
                    mm(ps[b2][0:96, 0:n], gwqs[:, k, h, :], cqT[:, k, cs], k == 0, k == 1, ["gwqs", ("cqT", c)], [PSK[b2]])
                tt("dve", Q[0:64, cs], ps[b1][0:64, 0:n], rqb[0:64, cs], ALU.mult, [PSK[b1], ("rqb", c)], [qk])
                tt("dve", rt[0][64:96, 0:n], ps[b1][64:96, 0:n], cos2[64:96, cs], ALU.mult, [PSK[b1], "const"], ["rt0"])
                tt("dve", rt[1][64:96, 0:n], ps[b2][64:96, 0:n], sin2[64:96, cs], ALU.mult, [PSK[b2], "const"], ["rt1"])
                mm(psb32[0:64, 0:n], gwk[:, h, :], ckvT[:, cs], True, True, ["gwk", ("ckvT", c)], ["psb"])
                tt("pool", rt[0][64:96, 0:n], rt[0][64:96, 0:n], rt[1][64:96, 0:n], ALU.add, ["rt0", "rt1"], ["rt0"])
                tt("pool", Q[64:96, cs], rt[0][64:96, 0:n], rqb[64:96, cs], ALU.mult, ["rt0", ("rqb", c)], [qk])
                tt("dve", K[0:64, cs], psb32[0:64, 0:n], rkb[0:64, cs], ALU.mult, ["psb", ("rkb", c)], [kk_])
                cp("act", K[64:96, cs], krot[64:96, cs], [("krot", c)], [kk_])

            def v_task(tiles):
                b = 3
                for i_, t in enumerate(tiles):
                    sz = TSZ[t]
                    mm(ps[b][0:sz, i_ * 64:(i_ + 1) * 64], ckvT[:, TOFF[t]:TOFF[t] + sz], gwv[:, h, :], True, True,
                       ["gwv", ("ckvT", TILE_CH[t])], [PSK[b]])
                for i_, t in enumerate(tiles):
                    sz = TSZ[t]
                    ts("dve", V[0:sz, t, 0:64], ps[b][0:sz, i_ * 64:(i_ + 1) * 64], rkp[0:sz, t:t + 1], None, ALU.mult, None,
                       [PSK[b], "rkp"], [vk])

            for c in range(NCH):
                tasks.append(lambda c=c: qk_task(c))
                tasks.append(lambda c=c: v_task(CH_TILES[c]))
            return tasks

        for tsk in mla_jit_tasks(0):
            tsk()
        for h in range(8):
            hb = h % 2
            Q, K, V = QmT[hb], KmT[hb], Vmh[hb]
            qk, kk_, vk = "QmT%d" % hb, "KmT%d" % hb, "Vmh%d" % hb
            hp = h % 2
            pr = slice(hp * 64, hp * 64 + 64)
            g = 2 + h // 2
            nxt = mla_jit_tasks(h + 1) if h + 1 < 8 else []
            if l == 0 and h == 0:
                tap("qm0", Q[0:96, 16:528], lambda d: d[:, :], [qk])
                tap("km0", K[0:96, 16:528], lambda d: d[:, :], [kk_])
            ml_steps = []
            for c in range(NCH):
                steps = chunk_steps(c, False)
                ob = 4 + (it[0] % 2)
                it[0] += 1
                for si, st_ in enumerate(steps):
                    ml_steps.append((c, ob, si, len(steps), st_))

            def ml_a(gi):
                c, ob, si, ns, (kt, co, n2, diag) = ml_steps[gi]
                sz = TSZ[kt]
                s3 = gi % 3
                qs = slice(COFF[c] + co, COFF[c] + co + n2)
                mm(ps[s3][0:sz, 0:n2], K[0:96, TOFF[kt]:TOFF[kt] + sz], Q[0:96, qs], True, True, [kk_, qk], [PSK[s3]])
                for _f in range(MLA_FILL):
                    mm(psb32[:, 0:512], zer[:, 0:128], zer[:, 0:512], True, True, ["const"], ["psb"])
                act(pt[s3][0:sz, 0:n2], ps[s3][0:sz, 0:n2], AF.Exp, [PSK[s3]], ["pt%d" % s3], scale=MLA_SCALE)
                if diag:
                    w_ = min(sz, 128)
                    tt("pool", pt[s3][0:sz, 0:w_], pt[s3][0:sz, 0:w_], maskle[0:sz, 0:w_], ALU.mult,
                       ["pt%d" % s3, "const"], ["pt%d" % s3])

            def ml_b(gi):
                c, ob, si, ns, (kt, co, n2, diag) = ml_steps[gi]
                sz = TSZ[kt]
                s3 = gi % 3
                n = CSZ[c]
                O = ps[ob]
                mm(O[:, co:co + n2], V[0:sz, kt, :], pt[s3][0:sz, 0:n2], si == 0, si == ns - 1,
                   [vk, "pt%d" % s3], [PSK[ob]])
                if si == ns - 1:
                    r2 = ob % 2
                    S.op("dve", lambda e, O=O, r2=r2, n=n: e.reciprocal(out=rec[r2][0:64, 0:n], in_=O[64:128, 0:n]), [PSK[ob]], ["rec%d" % r2])
                    tt("dve", U[pr, g, COFF[c]:COFF[c] + n], O[0:64, 0:n], rec[r2][0:64, 0:n], ALU.mult,
                       [PSK[ob], "rec%d" % r2], ukeys(c))

            NSK = MLA_SKEW
            for gi in range(len(ml_steps) + NSK):
                if gi < len(ml_steps):
                    ml_a(gi)
                if gi >= NSK:
                    ml_b(gi - NSK)
                if gi % 4 == 2 and nxt:
                    nxt.pop(0)()
            while nxt:
                nxt.pop(0)()
        if l == 0:
            tap("mlao", U[:, 2, 16:528], lambda d: d[:, :], ukeys(1))
        if stop == "MLA":
            break

        S.barrier()
        AR.off = markA
        cc = [AR.alloc([512], F32) for _ in range(2)]
        cb16 = [AR.alloc([512], BF16) for _ in range(2)]
        csq = [AR.alloc([512], BF16) for _ in range(4)]
        mean_s = AR.alloc([512], F32)
        rstd_s = AR.alloc([512], F32)
        lntmp = AR.alloc([512], F32)
        sv = [AR.alloc([512], F32) for _ in range(2)]
        for c in range(NCH):
            n = CSZ[c]
            cs = slice(COFF[c], COFF[c] + n)
            for j in range(2):
                b = j
                for k in range(31):
                    mm(ps[b][:, 0:n], convd[:, j, k, :], uT[:, j, COFF[c] + k:COFF[c] + k + n], k == 0, k == 30,
                       ["convd", "uTpad"] + [("uT", c2) for c2 in range(max(0, c - 1), c + 1)], [PSK[b]])
                act(cc[j][:, 0:n], ps[b][:, 0:n], AF.Identity, [PSK[b]], ["cc%d" % j], bias=colv[:, CB + j:CB + j + 1])
                cp("dve", cb16[j][:, 0:n], cc[j][:, 0:n], ["cc%d" % j], ["cb%d" % j])
                tt("pool", csq[j][:, 0:n], cc[j][:, 0:n], cc[j][:, 0:n], ALU.mult, ["cc%d" % j], ["csq%d" % j])
            for j in range(2):
                mm(ps[2][:, 0:n], c256[:, :], cb16[j][:, 0:n], j == 0, j == 1, ["const", "cb%d" % j], [PSK[2]])
            for j in range(2):
                mm(ps[3][:, 0:n], c256[:, :], csq[j][:, 0:n], j == 0, j == 1, ["const", "csq%d" % j], [PSK[3]])
            cp("act", mean_s[:, 0:n], ps[2][:, 0:n], [PSK[2]], ["mean_s"])
            tt("dve", rstd_s[:, 0:n], mean_s[:, 0:n], mean_s[:, 0:n], ALU.mult, ["mean_s"], ["rstd_s"])
            tt("dve", rstd_s[:, 0:n], ps[3][:, 0:n], rstd_s[:, 0:n], ALU.subtract, [PSK[3], "rstd_s"], ["rstd_s"])
            rsqrt(rstd_s[:, 0:n], rstd_s[:, 0:n], lntmp[:, 0:n], LN_EPS, ["rstd_s"], ["rstd_s"], "lntmp")
            for j in range(2):
                tt("pool", cc[j][:, 0:n], cc[j][:, 0:n], mean_s[:, 0:n], ALU.subtract, ["cc%d" % j, "mean_s"], ["cc%d" % j])
                tt("dve", cc[j][:, 0:n], cc[j][:, 0:n], rstd_s[:, 0:n], ALU.mult, ["cc%d" % j, "rstd_s"], ["cc%d" % j])
                ts("dve", cc[j][:, 0:n], cc[j][:, 0:n], colv[:, CLG + j:CLG + j + 1], colv[:, CLB + j:CLB + j + 1],
                   ALU.mult, ALU.add, ["cc%d" % j, "colv"], ["cc%d" % j])
                act(sv[j][:, 0:n], cc[j][:, 0:n], AF.Silu, ["cc%d" % j], ["sv%d" % j])
                tt("pool", csq[2 + j][:, 0:n], sv[j][:, 0:n], sv[j][:, 0:n], ALU.mult, ["sv%d" % j], ["csq%d" % (2 + j)])
            for j in range(2):
                mm(ps[4][:, 0:n], c256[:, :], csq[2 + j][:, 0:n], j == 0, j == 1, ["const", "csq%d" % (2 + j)], [PSK[4]])
            rsqrt(mean_s[:, 0:n], ps[4][:, 0:n], lntmp[:, 0:n], RMS_EPS, [PSK[4]], ["mean_s"], "lntmp")
            for j in range(2):
                stt(U[:, 6 + j, cs], sv[j][:, 0:n], colv[:, GG + 6 + j:GG + 7 + j], mean_s[:, 0:n], ALU.mult, ALU.mult,
                    ["sv%d" % j, "mean_s", "colv"], ukeys(c))
            for (g0, ng, cm, pb) in ((0, 2, c256, 5), (2, 4, c512, 6)):
                for gi in range(ng):
                    q_ = gi % 4
                    tt("pool", csq[q_][:, 0:n], U[:, g0 + gi, cs], U[:, g0 + gi, cs], ALU.mult, ukeys(c), ["csq%d" % q_])
                    mm(ps[pb][:, 0:n], cm[:, :], csq[q_][:, 0:n], gi == 0, gi == ng - 1, ["const", "csq%d" % q_], [PSK[pb]])
                rsqrt(rstd_s[:, 0:n], ps[pb][:, 0:n], lntmp[:, 0:n], RMS_EPS, [PSK[pb]], ["rstd_s"], "lntmp")
                for gi in range(ng):
                    stt(U[:, g0 + gi, cs], U[:, g0 + gi, cs], colv[:, GG + g0 + gi:GG + g0 + gi + 1], rstd_s[:, 0:n],
                        ALU.mult, ALU.mult, ukeys(c) + ["rstd_s", "colv"], ukeys(c))
        if l == 0:
            tap("yT", U[:, :, 16:528], lambda d: d.rearrange("(k p) n -> p k n", p=128), ukeys(1))
            tap("yT4", U[:, :, 1552:2064], lambda d: d.rearrange("(k p) n -> p k n", p=128), ukeys(4))
        S.barrier()
        if stop == "Y":
            break
        AR.off = markL

        wout = AR.alloc([8, D], BF16)
        rw = AR.alloc([8, NE], F32)
        rbb = AR.alloc([NE], F32)
        rwh = AR.alloc([8, NE], BF16)
        rwl = AR.alloc([8, NE], BF16)
        hlo = [AR.alloc([8, 128], BF16) for _ in range(2)]
        hold = [AR.alloc([D], F32) for _ in range(2)]
        rbuf = [AR.alloc([D], F32) for _ in range(2)]
        h1 = [AR.alloc([D], F32) for _ in range(2)]
        h1b = [AR.alloc([D], BF16) for _ in range(2)]
        hT32 = [AR.alloc([8, 128], F32) for _ in range(2)]
        st6 = [AR.alloc([12], F32) for _ in range(2)]
        mv = [AR.alloc([8], F32) for _ in range(2)]
        logit = [AR.alloc([NE], F32) for _ in range(2)]
        m8 = [AR.alloc([8], F32) for _ in range(2)]
        msk = [AR.alloc([NE], F32) for _ in range(2)]
        mskb = [AR.alloc([NE], BF16) for _ in range(2)]
        cum = AR.alloc([NE], BF16)
        et = [AR.alloc([NE], F32) for _ in range(2)]
        sm = [AR.alloc([8], F32) for _ in range(2)]
        t2 = [AR.alloc([NE], F32) for _ in range(2)]
        junk = [AR.alloc([NE], F32) for _ in range(2)]
        ridf = [AR.alloc([4], F32) for _ in range(2)]
        dma("pool", wout[:, :, :], P["w_out"][l].rearrange("(k p) n -> p k n", p=128), [], ["wout"], "wout")
        dma("sp", rw[:, :, :], P["router_w"][l].rearrange("(k p) n -> p k n", p=128), [], ["rw"], "rw")
        dma("sp", rbb[:, :], P["router_b"][l].partition_broadcast(128), [], ["rbb"], "rbb")
        cp("dve", rwh[:, :, :], rw[:, :, :], ["rw"], ["rwh"])
        tt("dve", rwl[:, :, :], rw[:, :, :], rwh[:, :, :], ALU.subtract, ["rw", "rwh"], ["rwl"])
        load_ln(P["ln_mix_g"][l], P["ln_mix_b"][l])
        S.op("pool", lambda e: e.memset(cum[:, :], 0.0), [], ["cum"])
        def d_front(t):
            s = t % 2
            sz = TSZ[t]
            dma("sp", hold[s][0:sz, :], hres[TOFF[t]:TOFF[t] + sz, :], [("hres", t)], ["hold%d" % s], "hold%d" % s)
            for half in range(2):
                b = half
                for k in range(8):
                    mm(ps[b][0:sz, :], U[:, k, TOFF[t]:TOFF[t] + sz], wout[:, k, half * 512:(half + 1) * 512], k == 0, k == 7,
                       ["wout", UK(t)], [PSK[b]])
                stt(rbuf[s][0:sz, half * 512:(half + 1) * 512], hold[s][0:sz, half * 512:(half + 1) * 512], ALPHA, ps[b][0:sz, :],
                    ALU.mult, ALU.add, ["hold%d" % s, PSK[b]], ["rbuf%d" % s])
            ln_stats(rbuf[s], sz, st6[s], mv[s], "rbuf%d" % s, "lnt%d" % s)

        def d_back(t):
            s = t % 2
            sz = TSZ[t]
            tk = "d%d" % s
            ln_apply(rbuf[s], h1[s], sz, mv[s], "rbuf%d" % s, "h1%d" % s, "lnt%d" % s)
            dma("sp", hres[TOFF[t]:TOFF[t] + sz, :], h1[s][0:sz, :], ["h1%d" % s], [("hres", t)], "h1%d" % s)
            cp("act", h1b[s][0:sz, :], h1[s][0:sz, :], ["h1%d" % s], ["h1b%d" % s])
            transpose_to_U(h1[s], sz, t, "h1%d" % s, want32=hT32[s], w32key="hT32%d" % s)
            if l == 0 and t == 1:
                tap("h1", h1[s][:, :], lambda d: d[:, :], ["h1%d" % s])
            if l == 0:
                tap("h1all", h1[s][0:sz, :], lambda d, t=t, sz=sz: d[TOFF[t]:TOFF[t] + sz, :], ["h1%d" % s])

        def d_back2(t):
            s = t % 2
            sz = TSZ[t]
            tk = "d%d" % s
            tt("dve", hlo[s][:, :, 0:sz], hT32[s][:, :, 0:sz], U[:, :, TOFF[t]:TOFF[t] + sz], ALU.subtract,
               ["hT32%d" % s, UK(t)], ["hlo%d" % s])
            for k in range(8):
                uk = U[:, k, TOFF[t]:TOFF[t] + sz]
                mm(ps[2][0:sz, 0:NE], uk, rwh[:, k, :], k == 0, False, [UK(t), "rwh"], [PSK[2]])
                mm(ps[2][0:sz, 0:NE], uk, rwl[:, k, :], False, False, [UK(t), "rwl"], [PSK[2]])
                mm(ps[2][0:sz, 0:NE], hlo[s][:, k, 0:sz], rwh[:, k, :], False, k == 7, ["hlo%d" % s, "rwh"], [PSK[2]])
            tt("dve", logit[s][0:sz, :], ps[2][0:sz, 0:NE], rbb[0:sz, :], ALU.add, [PSK[2], "rbb"], [tk])
            S.op("dve", lambda e, s=s, sz=sz: e.max(out=m8[s][0:sz, :], in_=logit[s][0:sz, :]), [tk], [tk])
            ts("dve", msk[s][0:sz, :], logit[s][0:sz, :], m8[s][0:sz, 3:4], None, ALU.is_ge, None, [tk], [tk])
            cp("dve", mskb[s][0:sz, :], msk[s][0:sz, :], [tk], ["mskb%d" % s])
            ts("dve", sm[s][0:sz, 0:1], m8[s][0:sz, 0:1], -1.0, None, ALU.mult, None, [tk], [tk])
            act(et[s][0:sz, :], logit[s][0:sz, :], AF.Exp, [tk], ["et%d" % s], bias=sm[s][0:sz, 0:1])
            stt(et[s][0:sz, :], et[s][0:sz, :], 1.0, msk[s][0:sz, :], ALU.mult, ALU.mult, ["et%d" % s, tk], ["et%d" % s],
                accum=sm[s][0:sz, 1:2])
            S.op("dve", lambda e, s=s, sz=sz: e.reciprocal(out=sm[s][0:sz, 2:3], in_=sm[s][0:sz, 1:2]), ["et%d" % s], [tk])
            ts("dve", Gt[0:sz, t, :], et[s][0:sz, :], sm[s][0:sz, 2:3], None, ALU.mult, None, ["et%d" % s, tk], [("G", t)])
            if DLEVEL <= 2:
                return
            mm(ps[3][0:sz, 0:NE], masks[0:sz, 0:sz], mskb[s][0:sz, :], True, t == 0, ["const", "mskb%d" % s], [PSK[3]])
            if t > 0:
                mm(ps[3][0:sz, 0:NE], c1[:, 0:sz], cum[:, :], False, True, ["const", "cum"], [PSK[3]])
            ts("dve", junk[s][0:sz, :], ps[3][0:sz, 0:NE], float(CAP), None, ALU.is_lt, None, [PSK[3]], [tk])
            tt("dve", t2[s][0:sz, :], ps[3][0:sz, 0:NE], ecol[0:sz, :], ALU.add, [PSK[3], "const"], [tk])
            tt("dve", t2[s][0:sz, :], t2[s][0:sz, :], junk[s][0:sz, :], ALU.mult, [tk], [tk])
            if t == 0:
                tt("pool", cum[0:sz, :], cum[0:sz, :], mskb[s][0:sz, :], ALU.add, ["cum", "mskb%d" % s], ["cum"])
            else:
                tt("pool", cum[:, :], cum[:, :], mskb[s][:, :], ALU.add, ["cum", "mskb%d" % s], ["cum"])
            for k in range(4):
                stt(junk[s][0:sz, :], logit[s][0:sz, :], m8[s][0:sz, k:k + 1], t2[s][0:sz, :], ALU.is_equal, ALU.mult, [tk], [tk],
                    accum=ridf[s][0:sz, k:k + 1])
                stt(junk[s][0:sz, :], logit[s][0:sz, :], m8[s][0:sz, k:k + 1], Gt[0:sz, t, :], ALU.is_equal, ALU.mult,
                    [tk, ("G", t)], [tk, ("gk", t)], accum=gk[0:sz, t, k:k + 1])
            ts("dve", ridi[0:sz, t, :], ridf[s][0:sz, :], float(EC), None, ALU.add, None, [tk], [("ridi", t)])
            for k in range(4 if not DEBUG_NO_SCATTER else 0):
                S.op("pool", lambda e, s=s, sz=sz, t=t, k=k: e.indirect_dma_start(
                    out=Xs[:, :], out_offset=bass.IndirectOffsetOnAxis(ap=ridi[0:sz, t, k:k + 1], axis=0),
                    in_=h1b[s][0:sz, :], in_offset=None),
                    ["h1b%d" % s, ("ridi", t)], [("Xs", t, k)], dma="h1b%d" % s)

        for t in range(NT + 2):
            if t < NT:
                d_front(t)
            if 1 <= t <= NT:
                d_back(t - 1)
            if t >= 2:
                d_back2(t - 2)
        tap("ridi%d" % l, ridi[:, :, :], lambda d: d.rearrange("p (t k) -> p t k", k=4), [("ridi", t) for t in range(NT)])
        if l == 0:
            tap("G", Gt[:, 1, :], lambda d: d[:, :], [("G", 1)])
            tap("gk", gk[:, 1, :], lambda d: d[:, :], [("gk", 1)])
        S.barrier()
        if stop == "D":
            break
        AR.off = markL

        NW = 2
        wgu = [AR.alloc([8, 2048], BF16) for _ in range(NW)]
        wd = [AR.alloc([8, D], BF16) for _ in range(NW)]
        bgu = AR.alloc([512], F32)
        bst = AR.alloc([4, 128], F32)
        xr = [AR.alloc([D], BF16) for _ in range(2)]
        XT = [AR.alloc([8, CAP], BF16) for _ in range(2)]
        actT = [AR.alloc([8, CAP], BF16) for _ in range(1)]
        gc = [AR.alloc([NQ], F32) for _ in range(2)]
        sgm = [AR.alloc([NQ], F32) for _ in range(2)]
        uc = [AR.alloc([NQ], F32) for _ in range(2)]
        bg7 = bgu
        yb = [AR.alloc([D], BF16) for _ in range(2)]
        bview = P["b_gate_up"][l].rearrange("e (c p) -> (e c) p", p=128)
        for q_ in range(4):
            dma("sp", bst[:, q_, :], bview[q_ * 128:(q_ + 1) * 128, :], [], ["bst"], "bst")
        for q_ in range(4):
            tr(ps[6][:, q_ * 128:(q_ + 1) * 128], bst[:, q_, :], ident[:, :], ["bst", "const"], [PSK[6]])
        cp("dve", bgu[:, :], ps[6][:, :], [PSK[6]], ["bgu"])
        bgv = bgu[:, :].rearrange("p (e c) -> p e c", e=NE)
        b7v = bg7[:, :].rearrange("p (e c) -> p e c", e=NE)
        ts("dve", b7v[:, :, 0:8], bgv[:, :, 0:8], -1.0, 7.0, ALU.mult, ALU.add, ["bgu"], ["bgu"])
        ts("dve", b7v[:, :, 8:16], bgv[:, :, 8:16], 7.0, None, ALU.add, None, ["bgu"], ["bgu", "bg7"])

        def load_w(e):
            s = e % NW
            for kh in range(2):
                dma("pool", wgu[s][:, kh * 4:kh * 4 + 4, :],
                    P["w_gate_up"][l, e, kh * 512:(kh + 1) * 512, :].rearrange("(k p) n -> p k n", p=128),
                    [], ["wgu%d_%d" % (s, kh)], "wgu%d_%d" % (s, kh))
            dma("pool", wd[s][:, :, :], P["w_down"][l, e].rearrange("(k p) n -> p k n", p=128), [], ["wd%d" % s], "wd%d" % s)

        load_w(0)
        xi = [0]
        yi = [0]
        def x_block(e, blk):
            xs_ = e % 2
            r = xi[0] % 2
            xi[0] += 1
            r0 = e * CAP + blk * 128
            dma("sp", xr[r][:, :], Xs[r0:r0 + 128, :], [], ["xr%d" % r], "xr%d" % r)
            for k in range(8):
                tr(psb[:, k * 128:(k + 1) * 128], xr[r][:, k * 128:(k + 1) * 128], identb[:, :], ["xr%d" % r, "const"], ["psb"])
            S.op("dve", lambda e_, xs_=xs_, blk=blk: e_.tensor_copy(out=XT[xs_][:, :, blk * 128:(blk + 1) * 128],
                                                                      in_=psb[:, :].rearrange("p (k t) -> p k t", k=8)), ["psb"], ["XT%d" % xs_])

        for blk in range(NBLK):
            x_block(0, blk)
        for e in range(NE):
            if e + 1 < NE:
                load_w(e + 1)
            s = e % NW
            xs_ = e % 2
            jq = 0
            for q0 in range(0, CAP, NQ):
              qs_ = slice(q0, q0 + NQ)
              for j in range(8):
                p2 = jq % 2
                jq += 1
                bg_, bu_ = 2 * p2, 2 * p2 + 1
                for k in range(8):
                    mm(ps[bg_][:, 0:NQ], wgu[s][:, k, j * 128:(j + 1) * 128], XT[xs_][:, k, qs_], k == 0, k == 7,
                       ["wgu%d_%d" % (s, k // 4), "XT%d" % xs_], [PSK[bg_]])
                for k in range(8):
                    mm(ps[bu_][:, 0:NQ], wgu[s][:, k, 1024 + j * 128:1024 + (j + 1) * 128], XT[xs_][:, k, qs_], k == 0, k == 7,
                       ["wgu%d_%d" % (s, k // 4), "XT%d" % xs_], [PSK[bu_]])
                act(gc[p2][:, :], ps[bg_][:, 0:NQ], AF.Relu, [PSK[bg_], "bg7"], ["gc%d" % p2],
                    bias=bg7[:, e * 16 + j:e * 16 + j + 1], scale=-1.0)
                act(sgm[p2][:, :], gc[p2][:, :], AF.Silu, ["gc%d" % p2], ["sgm%d" % p2], bias=silub[:, 0:1], scale=-1.702)
                act(uc[p2][:, :], ps[bu_][:, 0:NQ], AF.Relu, [PSK[bu_], "bg7"], ["uc%d" % p2],
                    bias=bg7[:, e * 16 + 8 + j:e * 16 + 9 + j], scale=1.0)
                ts("dve", uc[p2][:, :], uc[p2][:, :], 14.0, -6.0, ALU.min, ALU.add, ["uc%d" % p2], ["uc%d" % p2])
                stt(actT[0][:, j, qs_], sgm[p2][:, :], 1.0 / 1.702, uc[p2][:, :], ALU.mult, ALU.mult,
                    ["sgm%d" % p2, "uc%d" % p2], ["actT0"])
            for blk in range(NBLK):
                y_ = yi[0] % 2
                yi[0] += 1
                for half in range(2):
                    b = 4 + half
                    for j in range(8):
                        mm(ps[b][:, :], actT[0][:, j, blk * 128:(blk + 1) * 128], wd[s][:, j, half * 512:(half + 1) * 512],
                           j == 0, j == 7, ["actT0", "wd%d" % s], [PSK[b]])
                    if half == 0:
                        cp("act", yb[y_][:, 0:512], ps[b][:, :], [PSK[b]], ["yb%d" % y_])
                    else:
                        cp("dve", yb[y_][:, 512:1024], ps[b][:, :], [PSK[b]], ["yb%d" % y_])
                r0 = e * CAP + blk * 128
                dma("act", Ys[r0:r0 + 128, :], yb[y_][:, :], ["yb%d" % y_], [("Ys", e, blk)], "yb%d" % y_)
                if e + 1 < NE:
                    x_block(e + 1, blk)
        S.barrier()
        if stop == "E":
            break
        AR.off = markL

        bdn = AR.alloc([D], BF16)
        yg = [[AR.alloc([D], BF16) for _ in range(4)] for _ in range(3)]
        hold = [AR.alloc([D], F32) for _ in range(3)]
        acc = [AR.alloc([D], F32) for _ in range(2)]
        h2 = [AR.alloc([D], F32) for _ in range(2)]
        gT = [AR.alloc([128], BF16) for _ in range(3)]
        dg = [AR.alloc([4, 128], BF16) for _ in range(3)]
        st6 = [AR.alloc([12], F32) for _ in range(2)]
        mv = [AR.alloc([8], F32) for _ in range(2)]
        dma("pool", bdn[0:NE, :], P["b_down"][l], [], ["bdn"], "bdn")
        load_ln(P["ln_ffn_g"][l], P["ln_ffn_b"][l])
        ysk = [("Ys", e, blk) for e in range(NE) for blk in range(NBLK)] + ["Ys_dummy"]
        def f_pre(t):
            s = t % 3
            sz = TSZ[t]
            for k in range(4):
                S.op("pool", lambda e, s=s, sz=sz, t=t, k=k: e.indirect_dma_start(
                    out=yg[s][k][0:sz, :], out_offset=None, in_=Ys[:, :],
                    in_offset=bass.IndirectOffsetOnAxis(ap=ridi[0:sz, t, k:k + 1], axis=0),
                    ),
                    ysk + [("ridi", t)], ["yg%d_%d" % (s, k)], dma="yg%d_%d" % (s, k))
            dma("sp", hold[s][0:sz, :], hres[TOFF[t]:TOFF[t] + sz, :], [("hres", t)], ["hold%d" % s], "hold%d" % s)
            tr(ps[2][0:NE, 0:sz], Gt[0:sz, t, :], ident[0:sz, 0:sz], [("G", t), "const"], [PSK[2]])
            cp("act", gT[s][0:NE, 0:sz], ps[2][0:NE, 0:sz], [PSK[2]], ["gT%d" % s])
            for k in range(4):
                ts("dve", dg[s][0:sz, k, 0:sz], identb[0:sz, 0:sz], gk[0:sz, t, k:k + 1], None, ALU.mult, None,
                   ["const", ("gk", t)], ["dg%d" % s])

        def f_front(t):
            s = t % 2
            s3 = t % 3
            sz = TSZ[t]
            for half in range(2):
                b = half
                hs = slice(half * 512, (half + 1) * 512)
                mm(ps[b][0:sz, :], gT[s3][0:NE, 0:sz], bdn[0:NE, hs], True, False, ["gT%d" % s3, "bdn"], [PSK[b]])
                for k in range(4):
                    mm(ps[b][0:sz, :], dg[s3][0:sz, k, 0:sz], yg[s3][k][0:sz, hs], False, k == 3,
                       ["dg%d" % s3, "yg%d_%d" % (s3, k)], [PSK[b]])
                stt(acc[s][0:sz, hs], hold[s3][0:sz, hs], ALPHA, ps[b][0:sz, :],
                    ALU.mult, ALU.add, ["hold%d" % s3, PSK[b]], ["acc%d" % s])
            ln_stats(acc[s], sz, st6[s], mv[s], "acc%d" % s, "lnt%d" % s)

        def f_back(t):
            s = t % 2
            sz = TSZ[t]
            ln_apply(acc[s], h2[s], sz, mv[s], "acc%d" % s, "h2%d" % s, "lnt%d" % s)
            if l == 0 and t == 1:
                tap("h2", h2[s][:, :], lambda d: d[:, :], ["h2%d" % s])
            if l == 0:
                tap("h2all", h2[s][0:sz, :], lambda d, t=t, sz=sz: d[TOFF[t]:TOFF[t] + sz, :], ["h2%d" % s])
            if not last:
                dma("sp", hres[TOFF[t]:TOFF[t] + sz, :], h2[s][0:sz, :], ["h2%d" % s], [("hres", t)], "h2%d" % s)
                transpose_to_U(h2[s], sz, t, "h2%d" % s)
            elif t > 0:
                dma("sp", out_d[(t - 1) * 128:t * 128, :], h2[s][:, :], ["h2%d" % s], [("out", t)], "h2%d" % s)

        for t in range(NT + 2):
            if t < NT:
                f_pre(t)
            if 1 <= t <= NT:
                f_front(t - 1)
            if t >= 2:
                f_back(t - 2)
        S.barrier()
        AR.off = markL

    S.barrier()
    S.op("sp", None)

    S.finalize()
    import contextlib
    with contextlib.ExitStack() as es:
        esem = {e: es.enter_context(nc.semaphore("sem_" + e)) for e in Sched.ENG}
        dsem = {k: es.enter_context(nc.semaphore("dsem_%d" % i)) for i, k in enumerate(S.dma_cnt.keys())}
        block = es.enter_context(nc.Block())

        @block.tensor
        def _(e):
            S.emit("pe", e, esem, dsem)

        @block.scalar
        def _(e):
            S.emit("act", e, esem, dsem)

        @block.vector
        def _(e):
            S.emit("dve", e, esem, dsem)

        @block.gpsimd
        def _(e):
            S.emit("pool", e, esem, dsem)

        @block.sync
        def _(e):
            S.emit("sp", e, esem, dsem)
    info = dict(n_instr={e: len(S.q[e]) for e in Sched.ENG}, n_dsem=len(S.dma_cnt), arena_peak=AR.peak)
    return nc, info


_CACHE = {}


def kernel(**inputs):
    if "nc" not in _CACHE:
        _CACHE["nc"], _CACHE["info"] = build_program()
    nc = _CACHE["nc"]
    consts = host_consts()
    x = np.ascontiguousarray(np.asarray(inputs["x"], dtype=np.float32))
    shared = {name: np.ascontiguousarray(np.asarray(inputs[name], dtype=np.float32)) for name, _ in PARAM_SPECS}
    shared.update(consts)
    in_maps = []
    for b in range(8):
        m = dict(shared)
        m["x"] = x[b]
        in_maps.append(m)
    res = run_bass_kernel_spmd(nc, in_maps, core_ids=list(range(8)))
    return np.stack([np.asarray(r["out"], dtype=np.float32) for r in res.results], axis=0)
```

```python
import numpy as np
import concourse.bass as bass
import concourse.mybir as mybir
from concourse.bass_utils import run_bass_kernel_spmd

F32 = mybir.dt.float32
BF16 = mybir.dt.bfloat16
I32 = mybir.dt.int32
AF = mybir.ActivationFunctionType
ALU = mybir.AluOpType

D = 1024
SEQ = 2048
NMETA = 16
L = SEQ + NMETA
DEPTH = 2
NT = 17
TOFF = [0] + [NMETA + 128 * i for i in range(16)]
TSZ = [NMETA] + [128] * 16
NCH = 5
COFF = [0] + [NMETA + 512 * i for i in range(4)]
CSZ = [NMETA] + [512] * 4
CH_TILES = [[0]] + [list(range(4 * c + 1, 4 * c + 5)) for c in range(4)]
TILE_CH = [0] + [1 + i // 4 for i in range(16)]
NIN = 1696
NE = 32
CAP = 640
NQ = 320
NBLK = CAP // 128
EC = NE * CAP
ALPHA = float((2 * DEPTH) ** 0.25)
LN_EPS = 1e-5
RMS_EPS = 1e-6
SB_SCALE = 64 ** -0.5
MLA_SCALE = 96 ** -0.5
SAME_ENGINE_SYNC = True
DEBUG_NO_SCATTER = False
DLEVEL = 9
MLA_SKEW = 2
MLA_FILL = 0
SB_FILL = 0


class Sched:
    ENG = ("pe", "act", "dve", "pool", "sp")

    def __init__(self):
        self.q = {e: [] for e in self.ENG}
        self.state = {}
        self.dma_cnt = {}
        self.bar = None
        self.bar_pending = set()

    def op(self, eng, fn, reads=(), writes=(), dma=None):
        deps = {}

        def add(ev):
            src = (ev[0], ev[1])
            if deps.get(src, -1) < ev[2]:
                deps[src] = ev[2]

        for k in reads:
            st = self.state.get(k)
            if st is not None and st[0] is not None:
                add(st[0])
        for k in writes:
            st = self.state.get(k)
            if st is not None:
                if st[0] is not None:
                    add(st[0])
                for ev in st[1].values():
                    add(ev)
        if eng in self.bar_pending:
            for src, v in self.bar.items():
                if deps.get(src, -1) < v:
                    deps[src] = v
            self.bar_pending.discard(eng)
        idx = len(self.q[eng])
        if dma is None:
            ev = ("E", eng, idx)
        else:
            self.dma_cnt[dma] = self.dma_cnt.get(dma, 0) + 1
            ev = ("D", dma, self.dma_cnt[dma])
        for k in reads:
            st = self.state.setdefault(k, [None, {}])
            st[1][(ev[0], ev[1])] = ev
        for k in writes:
            self.state[k] = [ev, {}]
        self.q[eng].append(dict(fn=fn, deps=deps, dma=dma, inc=False))

    def barrier(self):
        bar = {}
        for e in self.ENG:
            if self.q[e]:
                bar[("E", e)] = len(self.q[e]) - 1
        for k, c in self.dma_cnt.items():
            bar[("D", k)] = c
        self.bar = bar
        self.bar_pending = set(self.ENG)

    def _skip(self, kind, src, eng):
        return kind == "E" and src == eng and (eng == "pe" or eng == "sp" or not SAME_ENGINE_SYNC)

    def finalize(self):
        for eng in self.ENG:
            for rec in self.q[eng]:
                for (kind, src), v in rec["deps"].items():
                    if kind == "E" and not self._skip(kind, src, eng):
                        self.q[src][v]["inc"] = True
        self.val = {}
        for eng in self.ENG:
            c = 0
            vals = []
            for rec in self.q[eng]:
                if rec["inc"] and rec["dma"] is None:
                    c += 1
                vals.append(c)
            self.val[eng] = vals

    def emit(self, eng, e, esem, dsem):
        seen = {}
        for rec in self.q[eng]:
            for (kind, src), v in rec["deps"].items():
                if self._skip(kind, src, eng):
                    continue
                if kind == "E":
                    value = self.val[src][v]
                    sem = esem[src]
                else:
                    value = 16 * v
                    sem = dsem[src]
                if seen.get((kind, src), -1) >= value:
                    continue
                seen[(kind, src)] = value
                e.wait_ge(sem, value)
            if rec["fn"] is None:
                continue
            ins = rec["fn"](e)
            if rec["dma"] is not None:
                ins.then_inc(dsem[rec["dma"]], 16)
            elif rec["inc"]:
                ins.then_inc(esem[eng], 1)


class Arena:
    def __init__(self, nc, nbytes):
        self.t = nc.alloc_sbuf_tensor("arena", [128, nbytes // 2], BF16).ap()
        self.cap = nbytes
        self.off = 0
        self.peak = 0

    def alloc(self, free, dtype):
        esz = 2 if dtype == BF16 else 4
        n = int(np.prod(free))
        nb = (n * esz + 31) // 32 * 32
        assert self.off + nb <= self.cap, ("arena overflow", self.off, nb, self.cap)
        a = self.t[:, self.off // 2:(self.off + n * esz) // 2]
        if esz == 4:
            a = a.bitcast(dtype)
        if len(free) == 2:
            a = a.rearrange("p (a b) -> p a b", a=free[0])
        elif len(free) == 3:
            a = a.rearrange("p (a b c) -> p a b c", a=free[0], b=free[1])
        self.off += nb
        self.peak = max(self.peak, self.off)
        return a


PARAM_SPECS = [
    ("meta_tokens", [16, 1024]), ("ln_in_g", [1024]), ("ln_in_b", [1024]),
    ("w_in", [2, 1024, 1696]), ("q_norm_g", [2, 256]), ("w_uq", [2, 256, 768]),
    ("kv_norm_g", [2, 128]), ("w_ukv", [2, 128, 1024]), ("conv_w", [2, 31, 256]),
    ("conv_b", [2, 256]), ("conv_ln_g", [2, 256]), ("conv_ln_b", [2, 256]),
    ("grp_norm_g", [2, 1024]), ("w_out", [2, 1024, 1024]), ("ln_mix_g", [2, 1024]),
    ("ln_mix_b", [2, 1024]), ("router_w", [2, 1024, 32]), ("router_b", [2, 32]),
    ("w_gate_up", [2, 32, 1024, 2048]), ("b_gate_up", [2, 32, 2048]),
    ("w_down", [2, 32, 1024, 1024]), ("b_down", [2, 32, 1024]),
    ("ln_ffn_g", [2, 1024]), ("ln_ffn_b", [2, 1024]),
]


def host_consts():
    c = {}
    c["c_ident"] = np.eye(128, dtype=np.float32)
    p = np.arange(128)
    c["c_tris"] = -(p[:, None] >= p[None, :]).astype(np.float32)
    c["c_masks"] = (p[:, None] < p[None, :]).astype(np.float32)
    c["c_maskle"] = (p[:, None] <= p[None, :]).astype(np.float32)
    inv = 1.0 / (10000.0 ** (np.arange(0, 32, 2, dtype=np.float32) / 32.0))
    ang = np.arange(L, dtype=np.float32)[None, :] * inv[:, None].astype(np.float32)
    cs = np.cos(ang).astype(np.float32)
    sn = np.sin(ang).astype(np.float32)
    c["c_cos2"] = np.concatenate([cs, cs], 0)
    c["c_sin2"] = np.concatenate([-sn, sn], 0)
    c["c_ecol"] = np.broadcast_to((np.arange(NE, dtype=np.float32) * CAP - EC)[None, :], (128, NE)).copy()
    return c


def build_program(taps=None, stop=None):
    taps = taps or {}
    nc = bass.Bass("TRN2", target_bir_lowering=False)
    S = Sched()
    dt_in = {}
    x_d = nc.dram_tensor("x", [SEQ, D], F32, kind="ExternalInput").ap()
    out_d = nc.dram_tensor("out", [SEQ, D], F32, kind="ExternalOutput").ap()
    P = {}
    for name, shp in PARAM_SPECS:
        P[name] = nc.dram_tensor(name, shp, F32, kind="ExternalInput").ap()
    CD = {}
    for name, arr in host_consts().items():
        CD[name] = nc.dram_tensor(name, list(arr.shape), F32, kind="ExternalInput").ap()
    tap_d = {}
    for name, shp in taps.items():
        tap_d[name] = nc.dram_tensor("tap_" + name, list(shp), F32, kind="ExternalOutput").ap()
    hres = nc.dram_tensor("hres", [L, D], F32).ap()
    Xs = nc.dram_tensor("xs_scr", [EC + 128, D], BF16).ap()
    Ys = nc.dram_tensor("ys_scr", [EC + 128, D], BF16).ap()

    AR = Arena(nc, 206 * 1024)
    ps = [nc.alloc_psum_tensor("ps%d" % i, [128, 512], F32).ap() for i in range(7)]
    psb32 = nc.alloc_psum_tensor("psb", [128, 512], F32).ap()
    psb = psb32[:, :].bitcast(BF16)
    PSK = ["ps%d" % i for i in range(7)]

    ident = AR.alloc([128], F32)
    identb = AR.alloc([128], BF16)
    tris = AR.alloc([128], BF16)
    masks = AR.alloc([128], BF16)
    maskle = AR.alloc([128], BF16)
    c1 = AR.alloc([128], BF16)
    c128 = AR.alloc([128], BF16)
    c256 = AR.alloc([128], BF16)
    c512 = AR.alloc([128], BF16)
    cm1 = AR.alloc([128], BF16)
    silub = AR.alloc([8], F32)
    zbig = AR.alloc([2, D], BF16)
    zer = AR.alloc([512], BF16)
    ecol = AR.alloc([NE], F32)
    U = AR.alloc([8, L], BF16)
    lng = AR.alloc([D], F32)
    lnb = AR.alloc([D], F32)
    Gt = AR.alloc([NT, NE], F32)
    ridi = AR.alloc([NT, 4], I32)
    gk = AR.alloc([NT, 4], F32)
    PERSIST = AR.off

    def UK(t):
        return ("U", t)

    UALL = [UK(t) for t in range(NT)]

    def ukeys(c):
        return [UK(t) for t in CH_TILES[c]]

    cnt = [0]

    def uid():
        cnt[0] += 1
        return cnt[0]

    def dma(eng, out, in_, reads, writes, sem):
        S.op(eng, lambda e: e.dma_start(out=out, in_=in_), reads, writes, dma=sem)

    def mm(out, lhsT, rhs, start, stop, reads, writes):
        S.op("pe", lambda e: e.matmul(out, lhsT, rhs, start=start, stop=stop), reads, writes)

    def tr(out, in_, idn, reads, writes):
        S.op("pe", lambda e: e.transpose(out, in_, idn), reads, writes)

    def act(out, in_, func, reads, writes, bias=0.0, scale=1.0, eng="act"):
        S.op("act", lambda e: e.activation(out=out, in_=in_, func=func, bias=bias, scale=scale), reads, writes)

    def tt(eng, out, a, b, op, reads, writes):
        S.op(eng, lambda e: e.tensor_tensor(out=out, in0=a, in1=b, op=op), reads, writes)

    def ts(eng, out, a, s1, s2, op0, op1, reads, writes):
        if op1 is None:
            S.op(eng, lambda e: e.tensor_scalar(out=out, in0=a, scalar1=s1, scalar2=None, op0=op0), reads, writes)
        else:
            S.op(eng, lambda e: e.tensor_scalar(out=out, in0=a, scalar1=s1, scalar2=s2, op0=op0, op1=op1), reads, writes)

    def stt(out, a, sc, b, op0, op1, reads, writes, accum=None):
        if accum is None:
            S.op("dve", lambda e: e.scalar_tensor_tensor(out=out, in0=a, scalar=sc, in1=b, op0=op0, op1=op1), reads, writes)
        else:
            S.op("dve", lambda e: e.scalar_tensor_tensor(out=out, in0=a, scalar=sc, in1=b, op0=op0, op1=op1, accum_out=accum), reads, writes)

    def cp(eng, out, in_, reads, writes):
        if eng == "act":
            S.op("act", lambda e: e.copy(out=out, in_=in_), reads, writes)
        else:
            S.op(eng, lambda e: e.tensor_copy(out=out, in_=in_), reads, writes)

    def rsqrt(out, in_, tmp, eps, reads, writes, tmpkey):
        act(tmp, in_, AF.Ln, reads, [tmpkey], bias=eps, scale=1.0)
        act(out, tmp, AF.Exp, [tmpkey], writes, scale=-0.5)

    def tap(name, src_ap, dst_slice, reads):
        if name in tap_d:
            dma("pool", dst_slice(tap_d[name]), src_ap, reads, ["tap_" + name], "tap_" + name)

    mark0 = AR.off
    stg = AR.alloc([128], F32)
    for nm, dst, val in (("c_tris", tris, None), ("c_masks", masks, None), ("c_maskle", maskle, None), ("c_ident", identb, None)):
        dma("sp", stg[:, :], CD[nm], [], ["stg"], "stg")
        cp("dve", dst[:, :], stg[:, :], ["stg"], ["const"])
    dma("sp", ident[:, :], CD["c_ident"], [], ["const"], "stg")
    dma("sp", ecol[:, :], CD["c_ecol"], [], ["const"], "stg")
    for dst, v in ((c1, 1.0), (c128, 1.0 / 128), (c256, 1.0 / 256), (c512, 1.0 / 512), (cm1, -1.0)):
        S.op("pool", lambda e, dst=dst, v=v: e.memset(dst[:, :], v), [], ["const"])
    S.op("pool", lambda e: e.memset(zer[:, :], 0.0), [], ["const"])
    S.op("pool", lambda e: e.memset(silub[:, :], 7.0 * 1.702), [], ["const"])
    S.op("pool", lambda e: e.memset(zbig[:, :, :], 0.0), [], ["const"])
    zrow = AR.alloc([D], BF16)
    S.op("pool", lambda e: e.memset(zrow[:, :], 0.0), [], ["zrow"])
    dma("sp", Ys[EC:EC + 128, :], zrow[:, :], ["zrow"], ["Ys_dummy"], "zrow")
    S.barrier()
    AR.off = mark0
    nz = (EC + 128) // 256
    for i in range(nz):
        dma("pool", Xs[i * 256:(i + 1) * 256, :].rearrange("(p a) d -> p a d", a=2), zbig[:, :, :], ["const"], [("Xs0", i)], "zbig")
    if (EC + 128) % 256:
        dma("pool", Xs[nz * 256:EC + 128, :], zbig[:, 0, :], ["const"], [("Xs0", nz)], "zbig")

    def load_ln(gap, bap):
        dma("sp", lng[:, :], gap.partition_broadcast(128), [], ["lng"], "lng")
        dma("sp", lnb[:, :], bap.partition_broadcast(128), [], ["lnb"], "lnb")

    def layer_norm(src, dst, sz, st6, mv, skey, dkey, tmpkey):
        ln_stats(src, sz, st6, mv, skey, tmpkey)
        ln_apply(src, dst, sz, mv, skey, dkey, tmpkey)

    def ln_apply(src, dst, sz, mv, skey, dkey, tmpkey):
        act(dst[0:sz, :], src[0:sz, :], AF.Identity, [skey, tmpkey], [dkey], bias=mv[0:sz, 3:4], scale=mv[0:sz, 2:3])
        tt("dve", dst[0:sz, :], dst[0:sz, :], lng[0:sz, :], ALU.mult, [dkey, "lng"], [dkey])
        tt("dve", dst[0:sz, :], dst[0:sz, :], lnb[0:sz, :], ALU.add, [dkey, "lnb"], [dkey])

    def ln_stats(src, sz, st6, mv, skey, tmpkey):
        S.op("dve", lambda e: e.bn_stats(out=st6[0:sz, 0:6], in_=src[0:sz, 0:512]), [skey], [tmpkey])
        S.op("dve", lambda e: e.bn_stats(out=st6[0:sz, 6:12], in_=src[0:sz, 512:1024]), [skey], [tmpkey])
        S.op("dve", lambda e: e.bn_aggr(out=mv[0:sz, 0:2], in_=st6[0:sz, 0:12]), [tmpkey], [tmpkey])
        rsqrt(mv[0:sz, 2:3], mv[0:sz, 1:2], mv[0:sz, 4:5], LN_EPS, [tmpkey], [tmpkey], tmpkey)
        stt(mv[0:sz, 3:4], mv[0:sz, 0:1], -1.0, mv[0:sz, 2:3], ALU.mult, ALU.mult, [tmpkey], [tmpkey])

    def transpose_to_U(src, sz, t, skey, want32=None, w32key=None):
        for half in range(2):
            pb = ps[5 + half]
            pk = PSK[5 + half]
            for kk in range(4):
                k = half * 4 + kk
                tr(pb[:, kk * 128:kk * 128 + sz], src[0:sz, k * 128:(k + 1) * 128], ident[0:sz, 0:sz], [skey, "const"], [pk])
            pv = pb[:, :].rearrange("p (k t) -> p k t", k=4)[:, :, 0:sz]
            if want32 is not None:
                S.op("dve", lambda e, pv=pv, half=half: e.tensor_copy(out=want32[:, half * 4:half * 4 + 4, 0:sz], in_=pv), [pk], [w32key])
                S.op("pool", lambda e, half=half: e.tensor_copy(out=U[:, half * 4:half * 4 + 4, TOFF[t]:TOFF[t] + sz],
                                                                 in_=want32[:, half * 4:half * 4 + 4, 0:sz]), [w32key], [UK(t)])
            else:
                S.op("dve", lambda e, pv=pv, half=half: e.tensor_copy(out=U[:, half * 4:half * 4 + 4, TOFF[t]:TOFF[t] + sz], in_=pv), [pk], [UK(t)])

    mark0 = AR.off
    xin = [AR.alloc([D], F32) for _ in range(2)]
    hn = [AR.alloc([D], F32) for _ in range(2)]
    st6 = [AR.alloc([12], F32) for _ in range(2)]
    mv = [AR.alloc([8], F32) for _ in range(2)]
    load_ln(P["ln_in_g"], P["ln_in_b"])
    def p0_front(t):
        s = t % 2
        sz = TSZ[t]
        src = P["meta_tokens"] if t == 0 else x_d[(t - 1) * 128:t * 128, :]
        dma("sp", xin[s][0:sz, :], src, [], ["xin%d" % s], "xin%d" % s)
        ln_stats(xin[s], sz, st6[s], mv[s], "xin%d" % s, "lnt%d" % s)

    def p0_back(t):
        s = t % 2
        sz = TSZ[t]
        ln_apply(xin[s], hn[s], sz, mv[s], "xin%d" % s, "hn%d" % s, "lnt%d" % s)
        dma("sp", hres[TOFF[t]:TOFF[t] + sz, :], hn[s][0:sz, :], ["hn%d" % s], [("hres", t)], "hn%d" % s)
        transpose_to_U(hn[s], sz, t, "hn%d" % s)
        if t == 1:
            tap("h0", hn[s][:, :], lambda d: d[0:128, :], ["hn%d" % s])

    for t in range(NT + 1):
        if t < NT:
            p0_front(t)
        if t >= 1:
            p0_back(t - 1)
    S.barrier()
    AR.off = mark0

    for l in range(DEPTH):
        last = l == DEPTH - 1
        markL = AR.off
        Qsb = AR.alloc([2, L], BF16)
        Ksb = AR.alloc([2, L], BF16)
        Vsb = AR.alloc([NT, 256], BF16)
        cqT = AR.alloc([2, L], BF16)
        ckvT = AR.alloc([L], BF16)
        krot = AR.alloc([L], BF16)
        uT = AR.alloc([2, L + 30], BF16)
        rqb = AR.alloc([L], BF16)
        rkb = AR.alloc([L], BF16)
        rkp = AR.alloc([NT], F32)
        colv = AR.alloc([79], F32)
        gwq = AR.alloc([2, 8, 96], BF16)
        gwqs = AR.alloc([2, 8, 96], BF16)
        gwk = AR.alloc([8, 64], BF16)
        gwv = AR.alloc([8, 64], BF16)
        convd = AR.alloc([2, 31, 128], BF16)
        cos2 = AR.alloc([L], BF16)
        sin2 = AR.alloc([L], BF16)
        for nm, dst in (("c_cos2", cos2), ("c_sin2", sin2)):
            for hf in range(2):
                dma("pool", dst[64:96, hf * 1032:(hf + 1) * 1032], CD[nm][:, hf * 1032:(hf + 1) * 1032], [], ["const"], "cs2")
        markA = AR.off
        win = AR.alloc([8, NIN], BF16)
        wkpe = AR.alloc([8, 2, 96], BF16)
        vst = AR.alloc([128], F32)
        wq32 = AR.alloc([2, 768], F32)
        wkv32 = AR.alloc([1024], F32)
        sgt = [AR.alloc([512], F32) for _ in range(2)]
        sqt = [AR.alloc([512], BF16) for _ in range(3)]
        rt = [AR.alloc([512], F32) for _ in range(3)]

        dma("pool", win[:, :, :], P["w_in"][l].rearrange("(k p) n -> p k n", p=128), [], ["win"], "win")
        S.op("pool", lambda e: e.memset(wkpe[:, :, :, :], 0.0), [], ["wkpe"])
        w_in_v = P["w_in"][l].rearrange("(k p) n -> p k n", p=128)
        dma("pool", wkpe[:, :, 0, 64:96], w_in_v[:, :, 1152:1184], [], ["wkpe"], "wkpe")
        dma("pool", wkpe[:, :, 1, 64:80], w_in_v[:, :, 1168:1184], [], ["wkpe"], "wkpe")
        dma("pool", wkpe[:, :, 1, 80:96], w_in_v[:, :, 1152:1168], [], ["wkpe"], "wkpe")
        S.op("pool", lambda e: e.memset(vst[:, :], 0.0), [], ["vst"])
        rows = [("conv_b", 0, 2), ("conv_ln_g", 2, 2), ("conv_ln_b", 4, 2), ("grp_norm_g", 6, 8),
                ("q_norm_g", 14, 2), ("kv_norm_g", 16, 1)]
        for nm, r0, nr in rows:
            dma("sp", vst[r0:r0 + nr, :], P[nm][l].rearrange("(r p) -> r p", p=128), [], ["vst"], "vst")
        dma("sp", vst[17:79, :], P["conv_w"][l].rearrange("k (j p) -> (k j) p", p=128), [], ["vst"], "vst")
        tr(ps[6][:, 0:79], vst[0:79, :], ident[0:79, 0:79], ["vst", "const"], [PSK[6]])
        cp("dve", colv[:, :], ps[6][:, 0:79], [PSK[6]], ["colv"])
        CB, CLG, CLB, GG, QG, KG, CW = 0, 2, 4, 6, 14, 16, 17
        dma("sp", wq32[:, :, :], P["w_uq"][l].rearrange("(k p) n -> p k n", p=128), [], ["wq32"], "wq32")
        S.op("pool", lambda e: e.memset(gwqs[:, :, :, :], 0.0), [], ["gwqs"])
        for k in range(2):
            wv = wq32[:, k, :].rearrange("p (h c) -> p h c", h=8)
            ts("dve", gwq[:, k, :, :], wv, colv[:, QG + k:QG + k + 1], None, ALU.mult, None, ["wq32", "colv"], ["gwq"])
            ts("dve", gwqs[:, k, :, 64:80], wv[:, :, 80:96], colv[:, QG + k:QG + k + 1], None, ALU.mult, None, ["wq32", "colv", "gwqs"], ["gwqs"])
            ts("dve", gwqs[:, k, :, 80:96], wv[:, :, 64:80], colv[:, QG + k:QG + k + 1], None, ALU.mult, None, ["wq32", "colv", "gwqs"], ["gwqs"])
        dma("sp", wkv32[:, :], P["w_ukv"][l], [], ["wkv32"], "wkv32")
        wkv = wkv32[:, :].rearrange("p (h c) -> p h c", h=8)
        ts("dve", gwk[:, :, :], wkv[:, :, 0:64], colv[:, KG:KG + 1], None, ALU.mult, None, ["wkv32", "colv"], ["gwk"])
        ts("dve", gwv[:, :, :], wkv[:, :, 64:128], colv[:, KG:KG + 1], None, ALU.mult, None, ["wkv32", "colv"], ["gwv"])
        for j in range(2):
            for k in range(31):
                ts("dve", convd[:, j, k, :], identb[:, :], colv[:, CW + 2 * k + j:CW + 2 * k + j + 1], None, ALU.mult, None,
                   ["const", "colv"], ["convd"])
        S.op("pool", lambda e: e.memset(uT[:, :, 0:30], 0.0), [], ["uTpad"])

        rr = [0]

        def nextps(n=5):
            b = rr[0] % n
            rr[0] += 1
            return b

        def proj_fm(c, col0, M, lhs_fn=None):
            b = nextps()
            n = CSZ[c]
            for k in range(8):
                lhsT = win[:, k, col0:col0 + M] if lhs_fn is None else lhs_fn(k)
                mm(ps[b][0:M, 0:n], lhsT, U[:, k, COFF[c]:COFF[c] + n], k == 0, k == 7,
                   ["win", "wkpe"] + ukeys(c), [PSK[b]])
            return b

        for c in range(NCH):
            n = CSZ[c]
            cs = slice(COFF[c], COFF[c] + n)
            for j in range(2):
                b = proj_fm(c, j * 128, 128)
                cp("act", Qsb[:, j, cs], ps[b][:, 0:n], [PSK[b]], [("Qsb", c)])
                b = proj_fm(c, 256 + j * 128, 128)
                cp("act", Ksb[:, j, cs], ps[b][:, 0:n], [PSK[b]], [("Ksb", c)])
            for j in range(2):
                b = proj_fm(c, 768 + j * 128, 128)
                cp("act", cqT[:, j, cs], ps[b][:, 0:n], [PSK[b]], [("cqT", c)])
                tt("pool", sqt[j][:, 0:n], cqT[:, j, cs], cqT[:, j, cs], ALU.mult, [("cqT", c)], ["sqt%d" % j])
            b = nextps()
            for j in range(2):
                mm(ps[b][:, 0:n], c256[:, :], sqt[j][:, 0:n], j == 0, j == 1, ["const", "sqt%d" % j], [PSK[b]])
            rsqrt(rqb[:, cs], ps[b][:, 0:n], rt[2][:, 0:n], RMS_EPS, [PSK[b]], [("rqb", c)], "rt2")
            b = proj_fm(c, 1024, 128)
            cp("act", ckvT[:, cs], ps[b][:, 0:n], [PSK[b]], [("ckvT", c)])
            tt("pool", sqt[2][:, 0:n], ckvT[:, cs], ckvT[:, cs], ALU.mult, [("ckvT", c)], ["sqt2"])
            b = nextps()
            mm(ps[b][:, 0:n], c128[:, :], sqt[2][:, 0:n], True, True, ["const", "sqt2"], [PSK[b]])
            rsqrt(rkb[:, cs], ps[b][:, 0:n], rt[2][:, 0:n], RMS_EPS, [PSK[b]], [("rkb", c)], "rt2")
            b = nextps()
            for t in CH_TILES[c]:
                o = TOFF[t] - COFF[c]
                mm(ps[b][0:TSZ[t], t:t + 1], sqt[2][:, o:o + TSZ[t]], c128[:, 0:1], True, True, ["const", "sqt2"], [PSK[b]])
            for t in CH_TILES[c]:
                rsqrt(rkp[0:TSZ[t], t:t + 1], ps[b][0:TSZ[t], t:t + 1], rt[2][0:TSZ[t], 0:1], RMS_EPS, [PSK[b]], ["rkp"], "rt2")
            b1 = proj_fm(c, 0, 96, lambda k: wkpe[:, k, 0, :])
            b2 = proj_fm(c, 0, 96, lambda k: wkpe[:, k, 1, :])
            tt("dve", rt[0][64:96, 0:n], ps[b1][64:96, 0:n], cos2[64:96, cs], ALU.mult, [PSK[b1], "const"], ["rt0"])
            tt("dve", rt[1][64:96, 0:n], ps[b2][64:96, 0:n], sin2[64:96, cs], ALU.mult, [PSK[b2], "const"], ["rt1"])
            tt("pool", krot[64:96, cs], rt[0][64:96, 0:n], rt[1][64:96, 0:n], ALU.add, ["rt0", "rt1"], [("krot", c)])
            for j in range(2):
                ba = proj_fm(c, 1184 + j * 128, 128)
                bg = proj_fm(c, 1440 + j * 128, 128)
                act(sgt[j][:, 0:n], ps[bg][:, 0:n], AF.Sigmoid, [PSK[bg]], ["sgt%d" % j])
                tt("dve", uT[:, j, 30 + COFF[c]:30 + COFF[c] + n], ps[ba][:, 0:n], sgt[j][:, 0:n], ALU.mult,
                   [PSK[ba], "sgt%d" % j], [("uT", c)])
            for t in CH_TILES[c]:
                b = nextps()
                sz = TSZ[t]
                for k in range(8):
                    mm(ps[b][0:sz, 0:256], U[:, k, TOFF[t]:TOFF[t] + sz], win[:, k, 512:768], k == 0, k == 7,
                       ["win", UK(t)], [PSK[b]])
                cp("act", Vsb[0:sz, t, :], ps[b][0:sz, 0:256], [PSK[b]], [("Vsb", t)])
        if l == 0:
            tap("qsb", Qsb[:, 0, 16:528], lambda d: d[:, :], [("Qsb", 1)])
            tap("cos2", cos2[64:96, :], lambda d: d[:, :], ["const"])
            tap("sin2", sin2[64:96, :], lambda d: d[:, :], ["const"])
            tap("krot", krot[64:96, 16:528], lambda d: d[:, :], [("krot", 1)])
            tap("uT", uT[:, 0, 30 + 16:30 + 528], lambda d: d[:, :], [("uT", 1)])
            tap("rqb", rqb[:, 16:528], lambda d: d[:, :], [("rqb", 1)])
        S.barrier()
        if stop == "A":
            break
        AR.off = markA

        markSB = AR.off
        sg = [[AR.alloc([512], F32) for _ in range(3)] for _ in range(2)]
        nl = [[AR.alloc([512], BF16) for _ in range(3)] for _ in range(2)]
        ex = [[AR.alloc([512], F32) for _ in range(2)] for _ in range(2)]
        wt = [[AR.alloc([512], BF16) for _ in range(2)] for _ in range(2)]
        Aacc = [AR.alloc([512], BF16) for _ in range(2)]

        def chunk_steps(c, descending):
            steps = []
            for kt in range(0, CH_TILES[c][-1] + 1):
                if kt in CH_TILES[c]:
                    co = TOFF[kt] - COFF[c]
                    steps.append((kt, co, CSZ[c] - co, True))
                else:
                    steps.append((kt, 0, CSZ[c], False))
            return steps[::-1] if descending else steps

        it = [0]
        sb_steps = []
        for j in range(2):
            for c in range(NCH):
                steps = chunk_steps(c, True)
                ob = it[0] % 2
                it[0] += 1
                for si, st_ in enumerate(steps):
                    sb_steps.append((j, c, ob, si, len(steps), st_))
        OB = [ps[6], psb32]
        OBK = [PSK[6], "psb"]

        def sb_stage_a(gi):
            j, c, ob, si, ns, (kt, co, n2, diag) = sb_steps[gi]
            sz = TSZ[kt]
            s3 = gi % 3
            kc = TILE_CH[kt]
            qs = slice(COFF[c] + co, COFF[c] + co + n2)
            for hp in range(2):
                pr = slice(hp * 64, hp * 64 + 64)
                zb = (gi % 2) * 2 + hp
                mm(ps[zb][0:sz, 0:n2], Ksb[pr, j, TOFF[kt]:TOFF[kt] + sz], Qsb[pr, j, qs], True, True,
                   [("Ksb", kc), ("Qsb", c)], [PSK[zb]])
            for hp in range(2):
                zb = (gi % 2) * 2 + hp
                k_ = "%d_%d" % (hp, s3)
                act(sg[hp][s3][0:sz, 0:n2], ps[zb][0:sz, 0:n2], AF.Exp, [PSK[zb]], ["sg" + k_], scale=SB_SCALE)
                if diag:
                    w_ = min(sz, 128)
                    tt("pool", sg[hp][s3][0:sz, 0:w_], sg[hp][s3][0:sz, 0:w_], masks[0:sz, 0:w_], ALU.mult,
                       ["sg" + k_, "const"], ["sg" + k_])
                act(nl[hp][s3][0:sz, 0:n2], sg[hp][s3][0:sz, 0:n2], AF.Ln, ["sg" + k_], ["nl" + k_], bias=1.0, scale=1.0)

        def sb_stage_b(gi):
            j, c, ob, si, ns, (kt, co, n2, diag) = sb_steps[gi]
            n = CSZ[c]
            sz = TSZ[kt]
            s3 = gi % 3
            s2 = gi % 2
            O = OB[ob]
            first = si == 0
            if first:
                mm(O[:, 0:n], zer[:, 0:128], zer[:, 0:n], True, False, ["const"], [OBK[ob]])
                for hp in range(2):
                    S.op("pool", lambda e, hp=hp: e.memset(Aacc[hp][:, :], 0.0), [], ["Aacc%d" % hp])
            for hp in range(2):
                cb = 4 + hp
                k_ = "%d_%d" % (hp, s3)
                mm(ps[cb][0:sz, 0:n2], tris[0:sz, 0:sz], nl[hp][s3][0:sz, 0:n2], True, first, ["const", "nl" + k_], [PSK[cb]])
                if not first:
                    mm(ps[cb][0:sz, 0:n2], cm1[:, 0:sz], Aacc[hp][:, co:co + n2], False, True, ["const", "Aacc%d" % hp], [PSK[cb]])
            for hp in range(2):
                cb = 4 + hp
                k_ = "%d_%d" % (hp, s3)
                k2 = "%d_%d" % (hp, s2)
                act(ex[hp][s2][0:sz, 0:n2], ps[cb][0:sz, 0:n2], AF.Exp, [PSK[cb]], ["ex" + k2])
                tt("dve", wt[hp][s2][0:sz, 0:n2], sg[hp][s3][0:sz, 0:n2], ex[hp][s2][0:sz, 0:n2], ALU.mult,
                   ["sg" + k_, "ex" + k2], ["wt" + k2])
                if si != ns - 1:
                    tt("pool", Aacc[hp][:, co:co + n2], Aacc[hp][:, co:co + n2], nl[hp][s3][:, 0:n2], ALU.add,
                       ["Aacc%d" % hp, "nl" + k_], ["Aacc%d" % hp])

        def sb_stage_c(gi):
            j, c, ob, si, ns, (kt, co, n2, diag) = sb_steps[gi]
            n = CSZ[c]
            sz = TSZ[kt]
            s2 = gi % 2
            O = OB[ob]
            for hp in range(2):
                h = 2 * j + hp
                k2 = "%d_%d" % (hp, s2)
                last_mm = si == ns - 1
                S.op("pe", lambda e, O=O, hp=hp, sz=sz, kt=kt, h=h, co=co, n2=n2, s2=s2, last_mm=last_mm: e.matmul(
                    O[hp * 64:hp * 64 + 64, co:co + n2], Vsb[0:sz, kt, h * 64:(h + 1) * 64], wt[hp][s2][0:sz, 0:n2],
                    start=False, stop=last_mm, tile_position=(0, hp * 64)),
                    [("Vsb", kt), "wt" + k2], [OBK[ob]])
            if si == ns - 1:
                cp("act", U[:, j, COFF[c]:COFF[c] + n], O[:, 0:n], [OBK[ob]], ukeys(c))

        for gi in range(len(sb_steps) + 2):
            if gi < len(sb_steps):
                sb_stage_a(gi)
            if 1 <= gi <= len(sb_steps):
                sb_stage_b(gi - 1)
            if gi >= 2:
                sb_stage_c(gi - 2)
        S.barrier()
        AR.off = markSB
        pt = [AR.alloc([512], BF16) for _ in range(3)]
        rec = [AR.alloc([512], F32) for _ in range(2)]
        rt = [AR.alloc([512], F32) for _ in range(2)]
        QmT = [AR.alloc([L], BF16) for _ in range(2)]
        KmT = [AR.alloc([L], BF16) for _ in range(2)]
        Vmh = [AR.alloc([NT, 128], BF16) for _ in range(2)]
        for i in range(2):
            S.op("pool", lambda e, i=i: e.memset(Vmh[i][:, :, 64:128], 1.0), [], ["Vmh%d" % i])
        if l == 0:
            tap("sbo", U[:, 0, 16:528], lambda d: d[:, :], ukeys(1))
        if stop == "SB":
            break

        def mla_jit_tasks(h):
            hb = h % 2
            Q, K, V = QmT[hb], KmT[hb], Vmh[hb]
            qk, kk_, vk = "QmT%d" % hb, "KmT%d" % hb, "Vmh%d" % hb
            tasks = []

            def qk_task(c):
                n = CSZ[c]
                cs = slice(COFF[c], COFF[c] + n)
                b1, b2 = 6, 3
                for k in range(2):
                    mm(ps[b1][0:96, 0:n], gwq[:, k, h, :], cqT[:, k, cs], k == 0, k == 1, ["gwq", ("cqT", c)], [PSK[b1]])
                for k in range(2):
                    mm(ps[b2][0:96, 0:n], gwqs[:, k, h, :], cqT[:, k, cs], k == 0, k == 1, ["gwqs", ("cqT", c)], [PSK[b2]])
                tt("dve", Q[0:64, cs], ps[b1][0:64, 0:n], rqb[0:64, cs], ALU.mult, [PSK[b1], ("rqb", c)], [qk])
                tt("dve", rt[0][64:96, 0:n], ps[b1][64:96, 0:n], cos2[64:96, cs], ALU.mult, [PSK[b1], "const"], ["rt0"])
                tt("dve", rt[1][64:96, 0:n], ps[b2][64:96, 0:n], sin2[64:96, cs], ALU.mult, [PSK[b2], "const"], ["rt1"])
                mm(psb32[0:64, 0:n], gwk[:, h, :], ckvT[:, cs], True, True, ["gwk", ("ckvT", c)], ["psb"])
                tt("pool", rt[0][64:96, 0:n], rt[0][64:96, 0:n], rt[1][64:96, 0:n], ALU.add, ["rt0", "rt1"], ["rt0"])
                tt("pool", Q[64:96, cs], rt[0][64:96, 0:n], rqb[64:96, cs], ALU.mult, ["rt0", ("rqb", c)], [qk])
                tt("dve", K[0:64, cs], psb32[0:64, 0:n], rkb[0:64, cs], ALU.mult, ["psb", ("rkb", c)], [kk_])
                cp("act", K[64:96, cs], krot[64:96, cs], [("krot", c)], [kk_])

            def v_task(tiles):
                b = 3
                for i_, t in enumerate(tiles):
                    sz = TSZ[t]
                    mm(ps[b][0:sz, i_ * 64:(i_ + 1) * 64], ckvT[:, TOFF[t]:TOFF[t] + sz], gwv[:, h, :], True, True,
                       ["gwv", ("ckvT", TILE_CH[t])], [PSK[b]])
                for i_, t in enumerate(tiles):
                    sz = TSZ[t]
                    ts("dve", V[0:sz, t, 0:64], ps[b][0:sz, i_ * 64:(i_ + 1) * 64], rkp[0:sz, t:t + 1], None, ALU.mult, None,
                       [PSK[b], "rkp"], [vk])

            for c in range(NCH):
                tasks.append(lambda c=c: qk_task(c))
                tasks.append(lambda c=c: v_task(CH_TILES[c]))
            return tasks

        for tsk in mla_jit_tasks(0):
            tsk()
        for h in range(8):
            hb = h % 2
            Q, K, V = QmT[hb], KmT[hb], Vmh[hb]
            qk, kk_, vk = "QmT%d" % hb, "KmT%d" % hb, "Vmh%d" % hb
            hp = h % 2
            pr = slice(hp * 64, hp * 64 + 64)
            g = 2 + h // 2
            nxt = mla_jit_tasks(h + 1) if h + 1 < 8 else []
            if l == 0 and h == 0:
                tap("qm0", Q[0:96, 16:528], lambda d: d[:, :], [qk])
                tap("km0", K[0:96, 16:528], lambda d: d[:, :], [kk_])
            ml_steps = []
            for c in range(NCH):
                steps = chunk_steps(c, False)
                ob = 4 + (it[0] % 2)
                it[0] += 1
                for si, st_ in enumerate(steps):
                    ml_steps.append((c, ob, si, len(steps), st_))

            def ml_a(gi):
                c, ob, si, ns, (kt, co, n2, diag) = ml_steps[gi]
                sz = TSZ[kt]
                s3 = gi % 3
                qs = slice(COFF[c] + co, COFF[c] + co + n2)
                mm(ps[s3][0:sz, 0:n2], K[0:96, TOFF[kt]:TOFF[kt] + sz], Q[0:96, qs], True, True, [kk_, qk], [PSK[s3]])
                for _f in range(MLA_FILL):
                    mm(psb32[:, 0:512], zer[:, 0:128], zer[:, 0:512], True, True, ["const"], ["psb"])
                act(pt[s3][0:sz, 0:n2], ps[s3][0:sz, 0:n2], AF.Exp, [PSK[s3]], ["pt%d" % s3], scale=MLA_SCALE)
                if diag:
                    w_ = min(sz, 128)
                    tt("pool", pt[s3][0:sz, 0:w_], pt[s3][0:sz, 0:w_], maskle[0:sz, 0:w_], ALU.mult,
                       ["pt%d" % s3, "const"], ["pt%d" % s3])

            def ml_b(gi):
                c, ob, si, ns, (kt, co, n2, diag) = ml_steps[gi]
                sz = TSZ[kt]
                s3 = gi % 3
                n = CSZ[c]
                O = ps[ob]
                mm(O[:, co:co + n2], V[0:sz, kt, :], pt[s3][0:sz, 0:n2], si == 0, si == ns - 1,
                   [vk, "pt%d" % s3], [PSK[ob]])
                if si == ns - 1:
                    r2 = ob % 2
                    S.op("dve", lambda e, O=O, r2=r2, n=n: e.reciprocal(out=rec[r2][0:64, 0:n], in_=O[64:128, 0:n]), [PSK[ob]], ["rec%d" % r2])
                    tt("dve", U[pr, g, COFF[c]:COFF[c] + n], O[0:64, 0:n], rec[r2][0:64, 0:n], ALU.mult,
                       [PSK[ob], "rec%d" % r2], ukeys(c))

            NSK = MLA_SKEW
            for gi in range(len(ml_steps) + NSK):
                if gi < len(ml_steps):
                    ml_a(gi)
                if gi >= NSK:
                    ml_b(gi - NSK)
                if gi % 4 == 2 and nxt:
                    nxt.pop(0)()
            while nxt:
                nxt.pop(0)()
        if l == 0:
            tap("mlao", U[:, 2, 16:528], lambda d: d[:, :], ukeys(1))
        if stop == "MLA":
            break

        S.barrier()
        AR.off = markA
        cc = [AR.alloc([512], F32) for _ in range(2)]
        cb16 = [AR.alloc([512], BF16) for _ in range(2)]
        csq = [AR.alloc([512], BF16) for _ in range(4)]
        mean_s = AR.alloc([512], F32)
        rstd_s = AR.alloc([512], F32)
        lntmp = AR.alloc([512], F32)
        sv = [AR.alloc([512], F32) for _ in range(2)]
        for c in range(NCH):
            n = CSZ[c]
            cs = slice(COFF[c], COFF[c] + n)
            for j in range(2):
                b = j
                for k in range(31):
                    mm(ps[b][:, 0:n], convd[:, j, k, :], uT[:, j, COFF[c] + k:COFF[c] + k + n], k == 0, k == 30,
                       ["convd", "uTpad"] + [("uT", c2) for c2 in range(max(0, c - 1), c + 1)], [PSK[b]])
                act(cc[j][:, 0:n], ps[b][:, 0:n], AF.Identity, [PSK[b]], ["cc%d" % j], bias=colv[:, CB + j:CB + j + 1])
                cp("dve", cb16[j][:, 0:n], cc[j][:, 0:n], ["cc%d" % j], ["cb%d" % j])
                tt("pool", csq[j][:, 0:n], cc[j][:, 0:n], cc[j][:, 0:n], ALU.mult, ["cc%d" % j], ["csq%d" % j])
            for j in range(2):
                mm(ps[2][:, 0:n], c256[:, :], cb16[j][:, 0:n], j == 0, j == 1, ["const", "cb%d" % j], [PSK[2]])
            for j in range(2):
                mm(ps[3][:, 0:n], c256[:, :], csq[j][:, 0:n], j == 0, j == 1, ["const", "csq%d" % j], [PSK[3]])
            cp("act", mean_s[:, 0:n], ps[2][:, 0:n], [PSK[2]], ["mean_s"])
            tt("dve", rstd_s[:, 0:n], mean_s[:, 0:n], mean_s[:, 0:n], ALU.mult, ["mean_s"], ["rstd_s"])
            tt("dve", rstd_s[:, 0:n], ps[3][:, 0:n], rstd_s[:, 0:n], ALU.subtract, [PSK[3], "rstd_s"], ["rstd_s"])
            rsqrt(rstd_s[:, 0:n], rstd_s[:, 0:n], lntmp[:, 0:n], LN_EPS, ["rstd_s"], ["rstd_s"], "lntmp")
            for j in range(2):
                tt("pool", cc[j][:, 0:n], cc[j][:, 0:n], mean_s[:, 0:n], ALU.subtract, ["cc%d" % j, "mean_s"], ["cc%d" % j])
                tt("dve", cc[j][:, 0:n], cc[j][:, 0:n], rstd_s[:, 0:n], ALU.mult, ["cc%d" % j, "rstd_s"], ["cc%d" % j])
                ts("dve", cc[j][:, 0:n], cc[j][:, 0:n], colv[:, CLG + j:CLG + j + 1], colv[:, CLB + j:CLB + j + 1],
                   ALU.mult, ALU.add, ["cc%d" % j, "colv"], ["cc%d" % j])
                act(sv[j][:, 0:n], cc[j][:, 0:n], AF.Silu, ["cc%d" % j], ["sv%d" % j])
                tt("pool", csq[2 + j][:, 0:n], sv[j][:, 0:n], sv[j][:, 0:n], ALU.mult, ["sv%d" % j], ["csq%d" % (2 + j)])
            for j in range(2):
                mm(ps[4][:, 0:n], c256[:, :], csq[2 + j][:, 0:n], j == 0, j == 1, ["const", "csq%d" % (2 + j)], [PSK[4]])
            rsqrt(mean_s[:, 0:n], ps[4][:, 0:n], lntmp[:, 0:n], RMS_EPS, [PSK[4]], ["mean_s"], "lntmp")
            for j in range(2):
                stt(U[:, 6 + j, cs], sv[j][:, 0:n], colv[:, GG + 6 + j:GG + 7 + j], mean_s[:, 0:n], ALU.mult, ALU.mult,
                    ["sv%d" % j, "mean_s", "colv"], ukeys(c))
            for (g0, ng, cm, pb) in ((0, 2, c256, 5), (2, 4, c512, 6)):
                for gi in range(ng):
                    q_ = gi % 4
                    tt("pool", csq[q_][:, 0:n], U[:, g0 + gi, cs], U[:, g0 + gi, cs], ALU.mult, ukeys(c), ["csq%d" % q_])
                    mm(ps[pb][:, 0:n], cm[:, :], csq[q_][:, 0:n], gi == 0, gi == ng - 1, ["const", "csq%d" % q_], [PSK[pb]])
                rsqrt(rstd_s[:, 0:n], ps[pb][:, 0:n], lntmp[:, 0:n], RMS_EPS, [PSK[pb]], ["rstd_s"], "lntmp")
                for gi in range(ng):
                    stt(U[:, g0 + gi, cs], U[:, g0 + gi, cs], colv[:, GG + g0 + gi:GG + g0 + gi + 1], rstd_s[:, 0:n],
                        ALU.mult, ALU.mult, ukeys(c) + ["rstd_s", "colv"], ukeys(c))
        if l == 0:
            tap("yT", U[:, :, 16:528], lambda d: d.rearrange("(k p) n -> p k n", p=128), ukeys(1))
            tap("yT4", U[:, :, 1552:2064], lambda d: d.rearrange("(k p) n -> p k n", p=128), ukeys(4))
        S.barrier()
        if stop == "Y":
            break
        AR.off = markL

        NW = 2
        bgu = AR.alloc([512], F32)
        bst = AR.alloc([4, 128], F32)
        xr = [AR.alloc([D], BF16) for _ in range(2)]
        XT = [AR.alloc([8, CAP], BF16) for _ in range(2)]
        actT = [AR.alloc([8, CAP], BF16) for _ in range(1)]
        gc = [AR.alloc([NQ], F32) for _ in range(2)]
        sgm = [AR.alloc([NQ], F32) for _ in range(2)]
        uc = [AR.alloc([NQ], F32) for _ in range(2)]
        bg7 = bgu
        yb = [AR.alloc([D], BF16) for _ in range(2)]
        wgu1 = AR.alloc([8, 2048], BF16)
        wd1 = AR.alloc([8, D], BF16)
        e_lo = AR.off
        wgu0 = AR.alloc([8, 2048], BF16)
        wd0 = AR.alloc([8, D], BF16)
        e_end = AR.off
        wgu = [wgu0, wgu1]
        wd = [wd0, wd1]
        AR.off = markL

        def load_w(e):
            s = e % NW
            for kh in range(2):
                dma("pool", wgu[s][:, kh * 4:kh * 4 + 4, :],
                    P["w_gate_up"][l, e, kh * 512:(kh + 1) * 512, :].rearrange("(k p) n -> p k n", p=128),
                    [], ["wgu%d_%d" % (s, kh)], "wgu%d_%d" % (s, kh))
            dma("pool", wd[s][:, :, :], P["w_down"][l, e].rearrange("(k p) n -> p k n", p=128), [], ["wd%d" % s], "wd%d" % s)

        wout = AR.alloc([8, D], BF16)
        rw = AR.alloc([8, NE], F32)
        rbb = AR.alloc([NE], F32)
        rwh = AR.alloc([8, NE], BF16)
        rwl = AR.alloc([8, NE], BF16)
        hlo = [AR.alloc([8, 128], BF16) for _ in range(2)]
        hold = [AR.alloc([D], F32) for _ in range(2)]
        rbuf = [AR.alloc([D], F32) for _ in range(2)]
        h1 = [AR.alloc([D], F32) for _ in range(2)]
        h1b = [AR.alloc([D], BF16) for _ in range(2)]
        hT32 = [AR.alloc([8, 128], F32) for _ in range(2)]
        st6 = [AR.alloc([12], F32) for _ in range(2)]
        mv = [AR.alloc([8], F32) for _ in range(2)]
        logit = [AR.alloc([NE], F32) for _ in range(2)]
        m8 = [AR.alloc([8], F32) for _ in range(2)]
        msk = [AR.alloc([NE], F32) for _ in range(2)]
        mskb = [AR.alloc([NE], BF16) for _ in range(2)]
        cum = AR.alloc([NE], BF16)
        et = [AR.alloc([NE], F32) for _ in range(2)]
        sm = [AR.alloc([8], F32) for _ in range(2)]
        t2 = [AR.alloc([NE], F32) for _ in range(2)]
        junk = [AR.alloc([NE], F32) for _ in range(2)]
        ridf = [AR.alloc([4], F32) for _ in range(2)]
        dma("pool", wout[:, :, :], P["w_out"][l].rearrange("(k p) n -> p k n", p=128), [], ["wout"], "wout")
        dma("sp", rw[:, :, :], P["router_w"][l].rearrange("(k p) n -> p k n", p=128), [], ["rw"], "rw")
        dma("sp", rbb[:, :], P["router_b"][l].partition_broadcast(128), [], ["rbb"], "rbb")
        cp("dve", rwh[:, :, :], rw[:, :, :], ["rw"], ["rwh"])
        tt("dve", rwl[:, :, :], rw[:, :, :], rwh[:, :, :], ALU.subtract, ["rw", "rwh"], ["rwl"])
        load_ln(P["ln_mix_g"][l], P["ln_mix_b"][l])
        S.op("pool", lambda e: e.memset(cum[:, :], 0.0), [], ["cum"])
        assert AR.off <= e_lo, (AR.off, e_lo)
        load_w(0)

        def d_front(t):
            s = t % 2
            sz = TSZ[t]
            dma("sp", hold[s][0:sz, :], hres[TOFF[t]:TOFF[t] + sz, :], [("hres", t)], ["hold%d" % s], "hold%d" % s)
            for half in range(2):
                b = half
                for k in range(8):
                    mm(ps[b][0:sz, :], U[:, k, TOFF[t]:TOFF[t] + sz], wout[:, k, half * 512:(half + 1) * 512], k == 0, k == 7,
                       ["wout", UK(t)], [PSK[b]])
                stt(rbuf[s][0:sz, half * 512:(half + 1) * 512], hold[s][0:sz, half * 512:(half + 1) * 512], ALPHA, ps[b][0:sz, :],
                    ALU.mult, ALU.add, ["hold%d" % s, PSK[b]], ["rbuf%d" % s])
            ln_stats(rbuf[s], sz, st6[s], mv[s], "rbuf%d" % s, "lnt%d" % s)

        def d_back(t):
            s = t % 2
            sz = TSZ[t]
            tk = "d%d" % s
            ln_apply(rbuf[s], h1[s], sz, mv[s], "rbuf%d" % s, "h1%d" % s, "lnt%d" % s)
            dma("sp", hres[TOFF[t]:TOFF[t] + sz, :], h1[s][0:sz, :], ["h1%d" % s], [("hres", t)], "h1%d" % s)
            cp("act", h1b[s][0:sz, :], h1[s][0:sz, :], ["h1%d" % s], ["h1b%d" % s])
            transpose_to_U(h1[s], sz, t, "h1%d" % s, want32=hT32[s], w32key="hT32%d" % s)
            if l == 0 and t == 1:
                tap("h1", h1[s][:, :], lambda d: d[:, :], ["h1%d" % s])
            if l == 0:
                tap("h1all", h1[s][0:sz, :], lambda d, t=t, sz=sz: d[TOFF[t]:TOFF[t] + sz, :], ["h1%d" % s])

        def d_back2(t):
            s = t % 2
            sz = TSZ[t]
            tk = "d%d" % s
            tt("dve", hlo[s][:, :, 0:sz], hT32[s][:, :, 0:sz], U[:, :, TOFF[t]:TOFF[t] + sz], ALU.subtract,
               ["hT32%d" % s, UK(t)], ["hlo%d" % s])
            for k in range(8):
                uk = U[:, k, TOFF[t]:TOFF[t] + sz]
                mm(ps[2][0:sz, 0:NE], uk, rwh[:, k, :], k == 0, False, [UK(t), "rwh"], [PSK[2]])
                mm(ps[2][0:sz, 0:NE], uk, rwl[:, k, :], False, False, [UK(t), "rwl"], [PSK[2]])
                mm(ps[2][0:sz, 0:NE], hlo[s][:, k, 0:sz], rwh[:, k, :], False, k == 7, ["hlo%d" % s, "rwh"], [PSK[2]])
            tt("dve", logit[s][0:sz, :], ps[2][0:sz, 0:NE], rbb[0:sz, :], ALU.add, [PSK[2], "rbb"], [tk])
            S.op("dve", lambda e, s=s, sz=sz: e.max(out=m8[s][0:sz, :], in_=logit[s][0:sz, :]), [tk], [tk])
            ts("dve", msk[s][0:sz, :], logit[s][0:sz, :], m8[s][0:sz, 3:4], None, ALU.is_ge, None, [tk], [tk])
            cp("dve", mskb[s][0:sz, :], msk[s][0:sz, :], [tk], ["mskb%d" % s])
            ts("dve", sm[s][0:sz, 0:1], m8[s][0:sz, 0:1], -1.0, None, ALU.mult, None, [tk], [tk])
            act(et[s][0:sz, :], logit[s][0:sz, :], AF.Exp, [tk], ["et%d" % s], bias=sm[s][0:sz, 0:1])
            stt(et[s][0:sz, :], et[s][0:sz, :], 1.0, msk[s][0:sz, :], ALU.mult, ALU.mult, ["et%d" % s, tk], ["et%d" % s],
                accum=sm[s][0:sz, 1:2])
            S.op("dve", lambda e, s=s, sz=sz: e.reciprocal(out=sm[s][0:sz, 2:3], in_=sm[s][0:sz, 1:2]), ["et%d" % s], [tk])
            ts("dve", Gt[0:sz, t, :], et[s][0:sz, :], sm[s][0:sz, 2:3], None, ALU.mult, None, ["et%d" % s, tk], [("G", t)])
            if DLEVEL <= 2:
                return
            mm(ps[3][0:sz, 0:NE], masks[0:sz, 0:sz], mskb[s][0:sz, :], True, t == 0, ["const", "mskb%d" % s], [PSK[3]])
            if t > 0:
                mm(ps[3][0:sz, 0:NE], c1[:, 0:sz], cum[:, :], False, True, ["const", "cum"], [PSK[3]])
            ts("dve", junk[s][0:sz, :], ps[3][0:sz, 0:NE], float(CAP), None, ALU.is_lt, None, [PSK[3]], [tk])
            tt("dve", t2[s][0:sz, :], ps[3][0:sz, 0:NE], ecol[0:sz, :], ALU.add, [PSK[3], "const"], [tk])
            tt("dve", t2[s][0:sz, :], t2[s][0:sz, :], junk[s][0:sz, :], ALU.mult, [tk], [tk])
            if t == 0:
                tt("pool", cum[0:sz, :], cum[0:sz, :], mskb[s][0:sz, :], ALU.add, ["cum", "mskb%d" % s], ["cum"])
            else:
                tt("pool", cum[:, :], cum[:, :], mskb[s][:, :], ALU.add, ["cum", "mskb%d" % s], ["cum"])
            for k in range(4):
                stt(junk[s][0:sz, :], logit[s][0:sz, :], m8[s][0:sz, k:k + 1], t2[s][0:sz, :], ALU.is_equal, ALU.mult, [tk], [tk],
                    accum=ridf[s][0:sz, k:k + 1])
                stt(junk[s][0:sz, :], logit[s][0:sz, :], m8[s][0:sz, k:k + 1], Gt[0:sz, t, :], ALU.is_equal, ALU.mult,
                    [tk, ("G", t)], [tk, ("gk", t)], accum=gk[0:sz, t, k:k + 1])
            ts("dve", ridi[0:sz, t, :], ridf[s][0:sz, :], float(EC), None, ALU.add, None, [tk], [("ridi", t)])
            for k in range(4 if not DEBUG_NO_SCATTER else 0):
                S.op("pool", lambda e, s=s, sz=sz, t=t, k=k: e.indirect_dma_start(
                    out=Xs[:, :], out_offset=bass.IndirectOffsetOnAxis(ap=ridi[0:sz, t, k:k + 1], axis=0),
                    in_=h1b[s][0:sz, :], in_offset=None),
                    ["h1b%d" % s, ("ridi", t)], [("Xs", t, k)], dma="h1b%d" % s)

        for t in range(NT + 2):
            if t < NT:
                d_front(t)
            if 1 <= t <= NT:
                d_back(t - 1)
            if t >= 2:
                d_back2(t - 2)
        tap("ridi%d" % l, ridi[:, :, :], lambda d: d.rearrange("p (t k) -> p t k", k=4), [("ridi", t) for t in range(NT)])
        if l == 0:
            tap("G", Gt[:, 1, :], lambda d: d[:, :], [("G", 1)])
            tap("gk", gk[:, 1, :], lambda d: d[:, :], [("gk", 1)])
        S.barrier()
        if stop == "D":
            break
        AR.off = markL

        AR.off = e_end
        bview = P["b_gate_up"][l].rearrange("e (c p) -> (e c) p", p=128)
        for q_ in range(4):
            dma("sp", bst[:, q_, :], bview[q_ * 128:(q_ + 1) * 128, :], [], ["bst"], "bst")
        for q_ in range(4):
            tr(ps[6][:, q_ * 128:(q_ + 1) * 128], bst[:, q_, :], ident[:, :], ["bst", "const"], [PSK[6]])
        cp("dve", bgu[:, :], ps[6][:, :], [PSK[6]], ["bgu"])
        bgv = bgu[:, :].rearrange("p (e c) -> p e c", e=NE)
        b7v = bg7[:, :].rearrange("p (e c) -> p e c", e=NE)
        ts("dve", b7v[:, :, 0:8], bgv[:, :, 0:8], -1.0, 7.0, ALU.mult, ALU.add, ["bgu"], ["bgu"])
        ts("dve", b7v[:, :, 8:16], bgv[:, :, 8:16], 7.0, None, ALU.add, None, ["bgu"], ["bgu", "bg7"])

        xi = [0]
        yi = [0]
        def x_block(e, blk):
            xs_ = e % 2
            r = xi[0] % 2
            xi[0] += 1
            r0 = e * CAP + blk * 128
            dma("sp", xr[r][:, :], Xs[r0:r0 + 128, :], [], ["xr%d" % r], "xr%d" % r)
            for k in range(8):
                tr(psb[:, k * 128:(k + 1) * 128], xr[r][:, k * 128:(k + 1) * 128], identb[:, :], ["xr%d" % r, "const"], ["psb"])
            S.op("dve", lambda e_, xs_=xs_, blk=blk: e_.tensor_copy(out=XT[xs_][:, :, blk * 128:(blk + 1) * 128],
                                                                      in_=psb[:, :].rearrange("p (k t) -> p k t", k=8)), ["psb"], ["XT%d" % xs_])

        for blk in range(NBLK):
            x_block(0, blk)
        for e in range(NE):
            if e + 1 < NE:
                load_w(e + 1)
            s = e % NW
            xs_ = e % 2
            jq = 0
            for q0 in range(0, CAP, NQ):
              qs_ = slice(q0, q0 + NQ)
              for j in range(8):
                p2 = jq % 2
                jq += 1
                bg_, bu_ = 2 * p2, 2 * p2 + 1
                for k in range(8):
                    mm(ps[bg_][:, 0:NQ], wgu[s][:, k, j * 128:(j + 1) * 128], XT[xs_][:, k, qs_], k == 0, k == 7,
                       ["wgu%d_%d" % (s, k // 4), "XT%d" % xs_], [PSK[bg_]])
                for k in range(8):
                    mm(ps[bu_][:, 0:NQ], wgu[s][:, k, 1024 + j * 128:1024 + (j + 1) * 128], XT[xs_][:, k, qs_], k == 0, k == 7,
                       ["wgu%d_%d" % (s, k // 4), "XT%d" % xs_], [PSK[bu_]])
                act(gc[p2][:, :], ps[bg_][:, 0:NQ], AF.Relu, [PSK[bg_], "bg7"], ["gc%d" % p2],
                    bias=bg7[:, e * 16 + j:e * 16 + j + 1], scale=-1.0)
                act(sgm[p2][:, :], gc[p2][:, :], AF.Silu, ["gc%d" % p2], ["sgm%d" % p2], bias=silub[:, 0:1], scale=-1.702)
                act(uc[p2][:, :], ps[bu_][:, 0:NQ], AF.Relu, [PSK[bu_], "bg7"], ["uc%d" % p2],
                    bias=bg7[:, e * 16 + 8 + j:e * 16 + 9 + j], scale=1.0)
                ts("dve", uc[p2][:, :], uc[p2][:, :], 14.0, -6.0, ALU.min, ALU.add, ["uc%d" % p2], ["uc%d" % p2])
                stt(actT[0][:, j, qs_], sgm[p2][:, :], 1.0 / 1.702, uc[p2][:, :], ALU.mult, ALU.mult,
                    ["sgm%d" % p2, "uc%d" % p2], ["actT0"])
            for blk in range(NBLK):
                y_ = yi[0] % 2
                yi[0] += 1
                for half in range(2):
                    b = 4 + half
                    for j in range(8):
                        mm(ps[b][:, :], actT[0][:, j, blk * 128:(blk + 1) * 128], wd[s][:, j, half * 512:(half + 1) * 512],
                           j == 0, j == 7, ["actT0", "wd%d" % s], [PSK[b]])
                    if half == 0:
                        cp("act", yb[y_][:, 0:512], ps[b][:, :], [PSK[b]], ["yb%d" % y_])
                    else:
                        cp("dve", yb[y_][:, 512:1024], ps[b][:, :], [PSK[b]], ["yb%d" % y_])
                r0 = e * CAP + blk * 128
                dma("act", Ys[r0:r0 + 128, :], yb[y_][:, :], ["yb%d" % y_], [("Ys", e, blk)], "yb%d" % y_)
                if e + 1 < NE:
                    x_block(e + 1, blk)
        S.barrier()
        if stop == "E":
            break
        AR.off = markL

        bdn = AR.alloc([D], BF16)
        yg = [[AR.alloc([D], BF16) for _ in range(4)] for _ in range(3)]
        hold = [AR.alloc([D], F32) for _ in range(3)]
        acc = [AR.alloc([D], F32) for _ in range(2)]
        h2 = [AR.alloc([D], F32) for _ in range(2)]
        gT = [AR.alloc([128], BF16) for _ in range(3)]
        dg = [AR.alloc([4, 128], BF16) for _ in range(3)]
        st6 = [AR.alloc([12], F32) for _ in range(2)]
        mv = [AR.alloc([8], F32) for _ in range(2)]
        dma("pool", bdn[0:NE, :], P["b_down"][l], [], ["bdn"], "bdn")
        load_ln(P["ln_ffn_g"][l], P["ln_ffn_b"][l])
        ysk = [("Ys", e, blk) for e in range(NE) for blk in range(NBLK)] + ["Ys_dummy"]
        def f_pre(t):
            s = t % 3
            sz = TSZ[t]
            for k in range(4):
                S.op("pool", lambda e, s=s, sz=sz, t=t, k=k: e.indirect_dma_start(
                    out=yg[s][k][0:sz, :], out_offset=None, in_=Ys[:, :],
                    in_offset=bass.IndirectOffsetOnAxis(ap=ridi[0:sz, t, k:k + 1], axis=0),
                    ),
                    ysk + [("ridi", t)], ["yg%d_%d" % (s, k)], dma="yg%d_%d" % (s, k))
            dma("sp", hold[s][0:sz, :], hres[TOFF[t]:TOFF[t] + sz, :], [("hres", t)], ["hold%d" % s], "hold%d" % s)
            tr(ps[2][0:NE, 0:sz], Gt[0:sz, t, :], ident[0:sz, 0:sz], [("G", t), "const"], [PSK[2]])
            cp("act", gT[s][0:NE, 0:sz], ps[2][0:NE, 0:sz], [PSK[2]], ["gT%d" % s])
            for k in range(4):
                ts("dve", dg[s][0:sz, k, 0:sz], identb[0:sz, 0:sz], gk[0:sz, t, k:k + 1], None, ALU.mult, None,
                   ["const", ("gk", t)], ["dg%d" % s])

        def f_front(t):
            s = t % 2
            s3 = t % 3
            sz = TSZ[t]
            for half in range(2):
                b = half
                hs = slice(half * 512, (half + 1) * 512)
                mm(ps[b][0:sz, :], gT[s3][0:NE, 0:sz], bdn[0:NE, hs], True, False, ["gT%d" % s3, "bdn"], [PSK[b]])
                for k in range(4):
                    mm(ps[b][0:sz, :], dg[s3][0:sz, k, 0:sz], yg[s3][k][0:sz, hs], False, k == 3,
                       ["dg%d" % s3, "yg%d_%d" % (s3, k)], [PSK[b]])
                stt(acc[s][0:sz, hs], hold[s3][0:sz, hs], ALPHA, ps[b][0:sz, :],
                    ALU.mult, ALU.add, ["hold%d" % s3, PSK[b]], ["acc%d" % s])
            ln_stats(acc[s], sz, st6[s], mv[s], "acc%d" % s, "lnt%d" % s)

        def f_back(t):
            s = t % 2
            sz = TSZ[t]
            ln_apply(acc[s], h2[s], sz, mv[s], "acc%d" % s, "h2%d" % s, "lnt%d" % s)
            if l == 0 and t == 1:
                tap("h2", h2[s][:, :], lambda d: d[:, :], ["h2%d" % s])
            if l == 0:
                tap("h2all", h2[s][0:sz, :], lambda d, t=t, sz=sz: d[TOFF[t]:TOFF[t] + sz, :], ["h2%d" % s])
            if not last:
                dma("sp", hres[TOFF[t]:TOFF[t] + sz, :], h2[s][0:sz, :], ["h2%d" % s], [("hres", t)], "h2%d" % s)
                transpose_to_U(h2[s], sz, t, "h2%d" % s)
            elif t > 0:
                dma("sp", out_d[(t - 1) * 128:t * 128, :], h2[s][:, :], ["h2%d" % s], [("out", t)], "h2%d" % s)

        for t in range(NT + 2):
            if t < NT:
                f_pre(t)
            if 1 <= t <= NT:
                f_front(t - 1)
            if t >= 2:
                f_back(t - 2)
        S.barrier()
        AR.off = markL

    S.barrier()
    S.op("sp", None)

    S.finalize()
    import contextlib
    with contextlib.ExitStack() as es:
        esem = {e: es.enter_context(nc.semaphore("sem_" + e)) for e in Sched.ENG}
        dsem = {k: es.enter_context(nc.semaphore("dsem_%d" % i)) for i, k in enumerate(S.dma_cnt.keys())}
        block = es.enter_context(nc.Block())

        @block.tensor
        def _(e):
            S.emit("pe", e, esem, dsem)

        @block.scalar
        def _(e):
            S.emit("act", e, esem, dsem)

        @block.vector
        def _(e):
            S.emit("dve", e, esem, dsem)

        @block.gpsimd
        def _(e):
            S.emit("pool", e, esem, dsem)

        @block.sync
        def _(e):
            S.emit("sp", e, esem, dsem)
    info = dict(n_instr={e: len(S.q[e]) for e in Sched.ENG}, n_dsem=len(S.dma_cnt), arena_peak=AR.peak)
    return nc, info


_CACHE = {}


def kernel(**inputs):
    if "nc" not in _CACHE:
        _CACHE["nc"], _CACHE["info"] = build_program()
    nc = _CACHE["nc"]
    consts = host_consts()
    x = np.ascontiguousarray(np.asarray(inputs["x"], dtype=np.float32))
    shared = {name: np.ascontiguousarray(np.asarray(inputs[name], dtype=np.float32)) for name, _ in PARAM_SPECS}
    shared.update(consts)
    in_maps = []
    for b in range(8):
        m = dict(shared)
        m["x"] = x[b]
        in_maps.append(m)
    res = run_bass_kernel_spmd(nc, in_maps, core_ids=list(range(8)))
    return np.stack([np.asarray(r["out"], dtype=np.float32) for r in res.results], axis=0)
```
